# Optimizing a Trainium2 kernel written in Bass

```python
import jax, jax.numpy as jnp
from jax import lax
import numpy as np

D_MODEL = 1024
BATCH = 4
SEQ = 4096
DEPTH = 1

PLE_DIM = 256
EPS = 1e-6
N_HEADS = 8
N_KV_HEADS = 2
HEAD_DIM = 64
ATTN_WIDTH = N_HEADS * HEAD_DIM
KV_WIDTH = N_KV_HEADS * HEAD_DIM
WINDOW = 128
BLOCK = WINDOW
ROPE_THETA = 10000.0
LRU_WIDTH = D_MODEL - ATTN_WIDTH
LRU_BLOCKS = 8
LRU_BLOCK_DIM = LRU_WIDTH // LRU_BLOCKS
CONV_WIDTH = 4
LRU_C = 8.0
MIX_WIDTH = ATTN_WIDTH + LRU_WIDTH
OFF_K = ATTN_WIDTH
OFF_V = OFF_K + KV_WIDTH
OFF_LX = OFF_V + KV_WIDTH
OFF_LG = OFF_LX + LRU_WIDTH
IN_PROJ_WIDTH = OFF_LG + LRU_WIDTH
N_GROUPS = 4
EXPERTS_PER_GROUP = 8
TOP_K = 2
EXPERT_FF = 512
NEG_INF = -1e30

kernel_name = "hymba_swa_rglru_hmoe_layer"


def rmsnorm(x, g):
    xf = x.astype(jnp.float32)
    y = xf * lax.rsqrt(jnp.mean(xf * xf, axis=-1, keepdims=True) + EPS)
    return (y * g.astype(jnp.float32)).astype(x.dtype)


def rope_tables(positions):
    inv_freq = ROPE_THETA ** (-jnp.arange(0, HEAD_DIM, 2, dtype=jnp.float32) / HEAD_DIM)
    ang = positions.astype(jnp.float32)[..., None] * inv_freq
    return jnp.cos(ang)[:, :, None, :], jnp.sin(ang)[:, :, None, :]


def apply_rope(t, cos, sin):
    tf = t.astype(jnp.float32)
    t1, t2 = tf[..., : HEAD_DIM // 2], tf[..., HEAD_DIM // 2:]
    out = jnp.concatenate([t1 * cos - t2 * sin, t2 * cos + t1 * sin], axis=-1)
    return out.astype(t.dtype)


def sliding_window_attention(q, k, v, sinks):
    B, S, H, Dh = q.shape
    nb = S // BLOCK
    grp = H // N_KV_HEADS
    qb = q.reshape(B, nb, BLOCK, N_KV_HEADS, grp, Dh)

    def with_prev_block(t):
        prev = jnp.pad(t, ((0, 0), (BLOCK, 0), (0, 0), (0, 0)))[:, :S]
        prev = prev.reshape(B, nb, BLOCK, N_KV_HEADS, Dh)
        cur = t.reshape(B, nb, BLOCK, N_KV_HEADS, Dh)
        return jnp.concatenate([prev, cur], axis=2)

    kb = with_prev_block(k)
    vb = with_prev_block(v)
    scale = HEAD_DIM ** -0.5
    scores = jnp.einsum('bnqkgd,bnskd->bnkgqs', qb, kb,
                        preferred_element_type=jnp.float32) * scale
    qi = jnp.arange(BLOCK)[:, None]
    kj = jnp.arange(2 * BLOCK)[None, :]
    rel = qi + BLOCK - kj
    band = (rel >= 0) & (rel < WINDOW)
    block_ok = (jnp.arange(nb)[:, None, None] > 0) | (kj >= BLOCK)[None]
    mask = band[None] & block_ok
    scores = jnp.where(mask[None, :, None, None], scores, NEG_INF)
    sink = sinks.astype(jnp.float32).reshape(N_KV_HEADS, grp)[None, None, :, :, None, None]
    sink = jnp.broadcast_to(sink, scores.shape[:-1] + (1,))
    probs = jax.nn.softmax(jnp.concatenate([scores, sink], axis=-1), axis=-1)[..., :-1]
    out = jnp.einsum('bnkgqs,bnskd->bnqkgd', probs.astype(vb.dtype), vb)
    return out.reshape(B, S, H * Dh)


def causal_depthwise_conv(x, w, b):
    y = lax.conv_general_dilated(
        x, w[:, None, :], window_strides=(1,), padding=((CONV_WIDTH - 1, 0),),
        dimension_numbers=('NWC', 'WIO', 'NWC'), feature_group_count=x.shape[-1])
    return y + b


def rg_lru(x, w_a, b_a, w_x, b_x, lam):
    B, S, C = x.shape
    xb = x.reshape(B, S, LRU_BLOCKS, LRU_BLOCK_DIM)
    gate_a = jnp.einsum('bshi,hij->bshj', xb, w_a).reshape(B, S, C) + b_a
    gate_x = jnp.einsum('bshi,hij->bshj', xb, w_x).reshape(B, S, C) + b_x
    r = jax.nn.sigmoid(gate_a.astype(jnp.float32))
    i = jax.nn.sigmoid(gate_x.astype(jnp.float32))
    log_a = -LRU_C * r * jax.nn.softplus(-lam.astype(jnp.float32))
    a = jnp.exp(log_a)
    mult = jnp.sqrt(-jnp.expm1(2.0 * log_a))
    bterm = mult * i * x.astype(jnp.float32)

    def combine(left, right):
        a1, b1 = left
        a2, b2 = right
        return a1 * a2, a2 * b1 + b2

    _, h = lax.associative_scan(combine, (a, bterm), axis=1)
    return h.astype(x.dtype)


def hierarchical_moe(h, w_rg, b_rg, w_re, b_re, w_gate, w_up, w_down):
    B, S, D = h.shape
    T = B * S
    t = h.reshape(T, D)
    g_prob = jax.nn.softmax((t @ w_rg).astype(jnp.float32) + b_rg.astype(jnp.float32), axis=-1)
    g_top_p, g_top = lax.top_k(g_prob, 1)
    e_logits = ((t @ w_re).astype(jnp.float32) + b_re.astype(jnp.float32)).reshape(
        T, N_GROUPS, EXPERTS_PER_GROUP)
    e_sel = jnp.take_along_axis(e_logits, g_top[:, :, None], axis=1)[:, 0]
    e_prob = jax.nn.softmax(e_sel, axis=-1)
    e_top_p, e_top = lax.top_k(e_prob, TOP_K)
    e_top_p = e_top_p / jnp.sum(e_top_p, axis=-1, keepdims=True)
    w_tok = g_top_p * e_top_p
    expert_w = jnp.sum(jax.nn.one_hot(e_top, EXPERTS_PER_GROUP, dtype=jnp.float32)
                       * w_tok[..., None], axis=1)
    combine = (jax.nn.one_hot(g_top[:, 0], N_GROUPS, dtype=jnp.float32)[:, :, None]
               * expert_w[:, None, :])
    out = jnp.zeros((T, D), dtype=h.dtype)
    for g in range(N_GROUPS):
        a = jnp.einsum('td,edf->tef', t, w_gate[g])
        u = jnp.einsum('td,edf->tef', t, w_up[g])
        hid = jax.nn.silu(a) * u * combine[:, g, :, None].astype(t.dtype)
        out = out + jnp.einsum('tef,efd->td', hid, w_down[g])
    return out.reshape(B, S, D)


def setup_inputs(seed: int = 0) -> dict:
    key = jax.random.key(seed)
    ks = jax.random.split(key, 32)
    f32 = jnp.float32

    def nrm(k, shape, scale):
        return jax.random.normal(k, shape, f32) * scale

    def gain(k, shape):
        return 1.0 + 0.01 * jax.random.normal(k, shape, f32)

    L, D, G, E, F = DEPTH, D_MODEL, N_GROUPS, EXPERTS_PER_GROUP, EXPERT_FF
    u = jax.random.uniform(ks[14], (L, LRU_WIDTH), f32, minval=0.9, maxval=0.999)
    s = u ** (1.0 / LRU_C)
    lru_lambda = jnp.log(s) - jnp.log1p(-s)
    positions = (jnp.arange(SEQ, dtype=jnp.int32)[None, :]
                 + jax.random.randint(ks[2], (BATCH, 1), 0, 1024, dtype=jnp.int32))
    return {
        "x": nrm(ks[0], (BATCH, SEQ, D), 1.0),
        "p": nrm(ks[1], (L, BATCH, SEQ, PLE_DIM), 1.0),
        "positions": positions,
        "g_mix": gain(ks[3], (L, D)),
        "w_in": nrm(ks[4], (L, D, IN_PROJ_WIDTH), D ** -0.5),
        "sinks": nrm(ks[5], (L, N_HEADS), 0.5),
        "conv_w": nrm(ks[6], (L, CONV_WIDTH, LRU_WIDTH), CONV_WIDTH ** -0.5),
        "conv_b": nrm(ks[7], (L, LRU_WIDTH), 0.01),
        "lru_wa": nrm(ks[8], (L, LRU_BLOCKS, LRU_BLOCK_DIM, LRU_BLOCK_DIM), LRU_BLOCK_DIM ** -0.5),
        "lru_ba": nrm(ks[9], (L, LRU_WIDTH), 0.01),
        "lru_wx": nrm(ks[10], (L, LRU_BLOCKS, LRU_BLOCK_DIM, LRU_BLOCK_DIM), LRU_BLOCK_DIM ** -0.5),
        "lru_bx": nrm(ks[11], (L, LRU_WIDTH), 0.01),
        "lru_lambda": lru_lambda,
        "g_attn_out": gain(ks[12], (L, ATTN_WIDTH)),
        "g_lru_out": gain(ks[13], (L, LRU_WIDTH)),
        "w_out": nrm(ks[15], (L, MIX_WIDTH, D), MIX_WIDTH ** -0.5),
        "g_ffn": gain(ks[16], (L, D)),
        "w_router_group": nrm(ks[17], (L, D, G), D ** -0.5),
        "b_router_group": nrm(ks[18], (L, G), 0.01),
        "w_router_expert": nrm(ks[19], (L, D, G * E), D ** -0.5),
        "b_router_expert": nrm(ks[20], (L, G * E), 0.01),
        "w_expert_gate": nrm(ks[21], (L, G, E, D, F), D ** -0.5),
        "w_expert_up": nrm(ks[22], (L, G, E, D, F), D ** -0.5),
        "w_expert_down": nrm(ks[23], (L, G, E, F, D), F ** -0.5),
        "g_ple": gain(ks[24], (L, D)),
        "w_ple_gate": nrm(ks[25], (L, D, D), D ** -0.5),
        "w_ple_proj": nrm(ks[26], (L, PLE_DIM, D), PLE_DIM ** -0.5),
        "g_final": gain(ks[27], (D,)),
    }


def reference(x, p, positions, g_mix, w_in, sinks, conv_w, conv_b, lru_wa, lru_ba,
              lru_wx, lru_bx, lru_lambda, g_attn_out, g_lru_out, w_out, g_ffn,
              w_router_group, b_router_group, w_router_expert, b_router_expert,
              w_expert_gate, w_expert_up, w_expert_down, g_ple, w_ple_gate, w_ple_proj,
              g_final):
    B, S, _ = x.shape
    cos, sin = rope_tables(positions)
    for i in range(DEPTH):
        h = rmsnorm(x, g_mix[i])
        proj = h @ w_in[i]
        q = proj[..., :OFF_K].reshape(B, S, N_HEADS, HEAD_DIM)
        k = proj[..., OFF_K:OFF_V].reshape(B, S, N_KV_HEADS, HEAD_DIM)
        v = proj[..., OFF_V:OFF_LX].reshape(B, S, N_KV_HEADS, HEAD_DIM)
        lx = proj[..., OFF_LX:OFF_LG]
        lg = proj[..., OFF_LG:]
        q = apply_rope(q, cos, sin)
        k = apply_rope(k, cos, sin)
        attn = sliding_window_attention(q, k, v, sinks[i])
        lx = causal_depthwise_conv(lx, conv_w[i], conv_b[i])
        lru = rg_lru(lx, lru_wa[i], lru_ba[i], lru_wx[i], lru_bx[i], lru_lambda[i])
        lru = lru * jax.nn.gelu(lg)
        mix = jnp.concatenate([rmsnorm(attn, g_attn_out[i]), rmsnorm(lru, g_lru_out[i])], axis=-1)
        x = x + mix @ w_out[i]
        h2 = rmsnorm(x, g_ffn[i])
        x = x + hierarchical_moe(h2, w_router_group[i], b_router_group[i],
                                 w_router_expert[i], b_router_expert[i],
                                 w_expert_gate[i], w_expert_up[i], w_expert_down[i])
        gate = jax.nn.sigmoid(rmsnorm(x, g_ple[i]) @ w_ple_gate[i])
        x = x + gate * (p[i] @ w_ple_proj[i])
    return rmsnorm(x, g_final)
```

```python
import numpy as np
from contextlib import ExitStack
import concourse.bass as bass
import concourse.mybir as mybir
from concourse.bass_utils import run_bass_kernel_spmd

F32 = mybir.dt.float32
BF16 = mybir.dt.bfloat16
I32 = mybir.dt.int32
AF = mybir.ActivationFunctionType
ALU = mybir.AluOpType
AX = mybir.AxisListType

ENGS = ("pe", "act", "dve", "pool", "sp")


class Buf:
    __slots__ = ("name", "writer", "readers", "dsem", "dcnt")

    def __init__(self, name):
        self.name = name
        self.writer = None
        self.readers = []
        self.dsem = None
        self.dcnt = 0


class Sched:
    def __init__(self, nc, stack):
        self.nc = nc
        self.stack = stack
        self.ops = {e: [] for e in ENGS}
        self.sems = {}
        self.cnt = {}
        for e in ("pe", "act", "dve", "pool"):
            self.sems[e] = stack.enter_context(nc.semaphore("s_" + e))
            self.cnt[e] = 0
        self.seen = {e: {} for e in ENGS}
        self.ndsem = 0
        self.nbuf = 0
        self.dtot = {}

    def buf(self, name=None):
        self.nbuf += 1
        return Buf(name or f"b{self.nbuf}")

    def _dsem(self, b):
        if b.dsem is None:
            self.ndsem += 1
            key = f"d{self.ndsem}"
            self.sems[key] = self.stack.enter_context(self.nc.semaphore("sd%d" % self.ndsem))
            b.dsem = key
            self.dtot[key] = 0
        return b.dsem

    def _collect(self, eng, reads, writes):
        need = {}

        def add(tok):
            if tok is None:
                return
            k, v = tok
            if eng == "pe" and k == "pe":
                return
            if need.get(k, 0) < v:
                need[k] = v

        for b in reads:
            add(b.writer)
        for b in writes:
            add(b.writer)
            for r in b.readers:
                add(r)
        waits = []
        seen = self.seen[eng]
        for k, v in need.items():
            if seen.get(k, 0) < v:
                seen[k] = v
                waits.append((k, v))
        return waits

    def _commit(self, tok, reads, writes):
        for b in writes:
            b.writer = tok
            b.readers = []
        for b in reads:
            if b not in writes:
                b.readers.append(tok)

    def op(self, eng, fn, reads=(), writes=()):
        waits = self._collect(eng, reads, writes)
        self.cnt[eng] += 1
        tok = (eng, self.cnt[eng])
        self.ops[eng].append((fn, waits, eng, 1))
        self._commit(tok, reads, writes)
        return tok

    def dma(self, q, out, in_, reads=(), writes=(), sembuf=None):
        sb = sembuf or (writes[0] if writes else reads[0])
        key = self._dsem(sb)
        waits = self._collect(q, reads, writes)
        sb.dcnt += 16
        self.dtot[key] += 16
        tok = (key, sb.dcnt)

        def fn(e, out=out, in_=in_):
            return e.dma_start(out=out, in_=in_)

        self.ops[q].append((fn, waits, key, 16))
        self._commit(tok, reads, writes)
        return tok

    def barrier(self):
        for e in ENGS:
            waits = []
            seen = self.seen[e]
            for k in ("pe", "act", "dve", "pool"):
                v = self.cnt[k]
                if k == e and e == "pe":
                    continue
                if v > 0 and seen.get(k, 0) < v:
                    seen[k] = v
                    waits.append((k, v))
            for k, v in self.dtot.items():
                if v > 0 and seen.get(k, 0) < v:
                    seen[k] = v
                    waits.append((k, v))
            if waits:
                self.ops[e].append((None, waits, None, 0))

    def emit(self):
        nc = self.nc
        self.barrier()
        sems = self.sems
        ops = self.ops

        def run(eng_name, e):
            for (fn, waits, key, amt) in ops[eng_name]:
                for (k, v) in waits:
                    e.wait_ge(sems[k], v)
                if fn is None:
                    continue
                ins = fn(e)
                ins.then_inc(sems[key], amt)

        with nc.Block() as block:
            @block.sync
            def _(e):
                run("sp", e)

            @block.scalar
            def _(e):
                run("act", e)

            @block.vector
            def _(e):
                run("dve", e)

            @block.gpsimd
            def _(e):
                run("pool", e)

            @block.tensor
            def _(e):
                run("pe", e)


class StopBuild(Exception):
    pass


class Region:
    def __init__(self, t, nwords):
        self.t = t
        self.n = nwords
        self.off = 0
        self.peak = 0

    def alloc(self, free_elems, dt=F32):
        esz = 4 if dt in (F32, I32) else 2
        words = (free_elems * esz + 3) // 4
        ap = self.t[:, self.off:self.off + words]
        self.off += words
        self.peak = max(self.peak, self.off)
        assert self.off <= self.n, f"region overflow {self.off} > {self.n}"
        return ap if dt == F32 else ap.bitcast(dt)


D = 1024
T_OWN = 2048
NT = 16
NPRE = 16
P = 128
OFF_K, OFF_V, OFF_LX, OFF_LG = 512, 640, 768, 1280
WIN = 1792
EPS = 1e-6
NEG = -1.0e9
NE = 32
TWO_PI = 6.283185307179586
GELU_C = 1.5957691216057308

DEBUG_STOP = None


def build(nc, dbg=None):
    dt = nc.dram_tensor
    x_d = dt("xin", [4096, D], F32, kind="ExternalInput").ap()
    pos_d = dt("pos", [P, 17], I32, kind="ExternalInput").ap()
    pp_d = dt("pple", [T_OWN, 256], F32, kind="ExternalInput").ap()
    win_d = dt("w_in", [D, WIN], F32, kind="ExternalInput").ap()
    wout_d = dt("w_out", [D, D], F32, kind="ExternalInput").ap()
    wrt_d = dt("w_rt", [D, 36], F32, kind="ExternalInput").ap()
    brt_d = dt("b_rt", [1, 36], F32, kind="ExternalInput").ap()
    ne_decl = NE if dbg in (None, "fewexp") else 1
    wg_d = dt("wg", [ne_decl, D, 512], F32, kind="ExternalInput").ap()
    wu_d = dt("wu", [ne_decl, D, 512], F32, kind="ExternalInput").ap()
    wd_d = dt("wd", [ne_decl, 512, D], F32, kind="ExternalInput").ap()
    wpg_d = dt("w_pg", [D, D], F32, kind="ExternalInput").ap()
    wpp_d = dt("w_pp", [256, D], F32, kind="ExternalInput").ap()
    gains_d = dt("gains", [4, D], F32, kind="ExternalInput").ap()
    colp_d = dt("colp", [P, 4, 10], F32, kind="ExternalInput").ap()
    wabd_d = dt("wa_bd", [P, 4, P], F32, kind="ExternalInput").ap()
    wxbd_d = dt("wx_bd", [P, 4, P], F32, kind="ExternalInput").ap()
    sinks_d = dt("sinks", [1, 8], F32, kind="ExternalInput").ap()
    mask0_d = dt("mask0", [P, 256], F32, kind="ExternalInput").ap()
    mask1_d = dt("mask1", [P, 256], F32, kind="ExternalInput").ap()
    hasp_d = dt("hasprev", [P, 1], F32, kind="ExternalInput").ap()
    ident_d = dt("ident", [P, P], F32, kind="ExternalInput").ap()
    invf_d = dt("invf", [P, 32], F32, kind="ExternalInput").ap()
    out_d = dt("out", [T_OWN, D], F32, kind="ExternalOutput").ap()

    with ExitStack() as st:
        S = Sched(nc, st)
        sb = lambda name, shape, dtp=F32: st.enter_context(nc.sbuf_tensor("sb_" + name, shape, dtp))
        B = S.buf

        X = sb("X", [P, NT, D])
        Xb = [B(f"X{i}") for i in range(NT)]
        gbc = sb("gbc", [P, D]); gbc_b = B("gbc")
        colp = sb("colp", [P, 4, 10]); colp_b = B("colp")
        der = sb("der", [P, 4, 8]); der_b = B("der")
        id16 = sb("id16", [P, P], BF16); id16_b = B("id16")
        id32 = sb("id32", [P, P]); id32_b = B("id32")
        ones16 = sb("ones16", [P, 2], BF16); ones_b = B("ones")
        sinks = sb("sinksb", [P, 8]); sinks_b = B("sinks")
        hasp = sb("hasp", [P, 1]); hasp_b = B("hasp")
        small = sb("small", [P, 64]); small_b = B("small")
        scr16 = sb("scr16", [P, D], BF16); scr_b = B("scr16")
        RW = 34900
        Rt = sb("R", [P, RW])
        R = Region(Rt, RW)

        ps = [st.enter_context(nc.psum_tensor(f"ps{i}", [P, 512], F32)) for i in range(8)]
        pb = [B(f"ps{i}") for i in range(8)]

        S.dma("sp", colp[:], colp_d, writes=[colp_b])
        S.dma("sp", id32[:], ident_d, writes=[id32_b])
        S.dma("pool", id16[:], ident_d, writes=[id16_b])
        S.dma("sp", sinks[:], sinks_d.partition_broadcast(P), writes=[sinks_b])
        S.dma("sp", hasp[:], hasp_d, writes=[hasp_b])
        S.dma("sp", gbc[:], gains_d[0:1, :].partition_broadcast(P), writes=[gbc_b])
        S.op("dve", lambda e: e.memset(ones16[:], 1.0), writes=[ones_b])

        S.op("act", lambda e: e.activation(der[:, :, 4], colp[:, :, 7], AF.Exp, scale=-1.0),
             reads=[colp_b], writes=[der_b])
        S.op("act", lambda e: e.activation(der[:, :, 5], der[:, :, 4], AF.Ln, bias=1.0),
             reads=[der_b], writes=[der_b])
        S.op("dve", lambda e: e.tensor_scalar(der[:, :, 0], der[:, :, 5], -8.0, None, ALU.mult),
             reads=[der_b], writes=[der_b])
        S.op("dve", lambda e: e.tensor_scalar(der[:, :, 1], der[:, :, 5], -16.0, None, ALU.mult),
             reads=[der_b], writes=[der_b])
        S.op("dve", lambda e: e.tensor_scalar(der[:, :, 2], colp[:, :, 5], -1.0, None, ALU.mult),
             reads=[colp_b, der_b], writes=[der_b])
        S.op("dve", lambda e: e.tensor_scalar(der[:, :, 3], colp[:, :, 6], -1.0, None, ALU.mult),
             reads=[colp_b, der_b], writes=[der_b])

        Win = R.alloc(8 * WIN, BF16).rearrange("p (c n) -> p c n", c=8); Win_b = B("Win")
        Wout = R.alloc(8 * D, BF16).rearrange("p (c n) -> p c n", c=8); Wout_b = B("Wout")
        wabd = R.alloc(4 * P, BF16).rearrange("p (c n) -> p c n", c=4)
        wxbd = R.alloc(4 * P, BF16).rearrange("p (c n) -> p c n", c=4)
        wbd_b = B("wbd")
        hT = R.alloc(8 * 512, BF16).rearrange("p (c t) -> p c t", c=8); hT_b = B("hT")
        hbf = R.alloc(D, BF16); hbf_b = B("hbf")
        hbf2 = R.alloc(D, BF16); hbf2_b = B("hbf2")
        smn0 = R.alloc(4); smn0_b = B("smn0"); smn1 = R.alloc(4); smn1_b = B("smn1")
        lxb = R.alloc(4 * 515).rearrange("p (c t) -> p c t", c=4); lxb_b = [B(f"lxb{c}") for c in range(4)]
        state = R.alloc(4); state_b = B("state")
        cvs = [R.alloc(512) for _ in range(3)]; cvs_b = [B("cv0"), B("cv1"), B("cv2")]
        cvbs = [R.alloc(512, BF16) for _ in range(3)]; cvbs_b = [B("cvb0"), B("cvb1"), B("cvb2")]
        t2x = R.alloc(512); t2x_b = B("t2x"); t3x = R.alloc(512); t3x_b = B("t3x")
        NTMP = 7
        off_alias = R.off
        ang = R.alloc(17 * 32); angi = R.alloc(17 * 32, I32); ang2 = R.alloc(17 * 32)
        ang_b = B("ang")
        R.off = off_alias
        tmp = [R.alloc(512) for _ in range(NTMP)]; tmp_b = [B(f"tmp{i}") for i in range(NTMP)]
        hl = R.alloc(512); hl_b = B("hl")
        hl2 = R.alloc(512); hl2_b = B("hl2")
        yT = R.alloc(4 * 512, BF16).rearrange("p (c t) -> p c t", c=4); yT_b = B("yT")
        sqT = R.alloc(4 * 512, BF16).rearrange("p (c t) -> p c t", c=4); sqT_b = B("sqT")
        sslru = R.alloc(4); sslru_b = B("sslru")
        cos_t = R.alloc(17 * 32).rearrange("p (t j) -> p t j", t=17)
        sin_t = R.alloc(17 * 32).rearrange("p (t j) -> p t j", t=17)
        nsin_t = R.alloc(17 * 32).rearrange("p (t j) -> p t j", t=17)
        rope_b = B("rope")
        posi = R.alloc(17, I32); posf = R.alloc(17); pos_b = B("pos")
        invf = R.alloc(32); invf_b = B("invf")
        mask0 = R.alloc(256); mask1 = R.alloc(256); mask_b = B("mask")
        qk_sb = R.alloc(640); qk_b = B("qk_sb")
        rA = R.alloc(640); rB = R.alloc(640); rt_b = B("ropetmp")
        qkr = R.alloc(640, BF16); qkr_b = B("qkr")
        qT = R.alloc(8 * P, BF16).rearrange("p (c t) -> p c t", c=8); qT_b = B("qT")
        kT = [R.alloc(2 * P, BF16).rearrange("p (c t) -> p c t", c=2) for _ in range(3)]
        kT_b = [B("kT0"), B("kT1"), B("kT2")]
        vv = [R.alloc(P, BF16) for _ in range(3)]; vv_b = [B("v0"), B("v1"), B("v2")]
        p_sbs = [R.alloc(2 * 256, BF16).rearrange("p (h k) -> p h k", h=2) for _ in range(2)]; p_bs = [B("p_sb0"), B("p_sb1")]
        pTs = [R.alloc(4 * P, BF16).rearrange("p (h t) -> p h t", h=4) for _ in range(2)]; pT_bs = [B("pT0"), B("pT1")]
        maskb0 = R.alloc(256, BF16); maskb1 = R.alloc(256, BF16); maskb_b = B("maskb")
        sinks8 = R.alloc(8)
        sm2 = R.alloc(64); sm2_b = B("sm2")
        attn = R.alloc(512); attn_b = B("attn")
        attnb = R.alloc(512, BF16); attnb_b = B("attnb")
        attnT = R.alloc(4 * P, BF16).rearrange("p (c t) -> p c t", c=4); attnT_b = B("attnT")
        sm = R.alloc(64); sm_b = B("sm")
        rinv8t = R.alloc(8); rinv_b = B("rinv")
        print("phase1 region words", R.off, "of", RW)

        S.dma("pool", Win[:, :, OFF_LX:OFF_LG], win_d[:, OFF_LX:OFF_LG].rearrange("(c p) n -> p c n", p=P), writes=[Win_b])
        S.dma("pool", wabd[:], wabd_d, writes=[wbd_b])
        S.dma("pool", wxbd[:], wxbd_d, writes=[wbd_b])
        S.dma("pool", Win[:, :, 0:OFF_LX], win_d[:, 0:OFF_LX].rearrange("(c p) n -> p c n", p=P), writes=[Win_b])
        S.dma("pool", Win[:, :, OFF_LG:WIN], win_d[:, OFF_LG:WIN].rearrange("(c p) n -> p c n", p=P), writes=[Win_b])
        S.dma("sp", posi[:], pos_d, writes=[pos_b])
        S.dma("sp", invf[:], invf_d, writes=[invf_b])
        S.dma("sp", mask0[:], mask0_d, writes=[mask_b])
        S.dma("sp", mask1[:], mask1_d, writes=[mask_b])

        S.op("dve", lambda e: e.tensor_scalar(maskb0[:], mask0[:], 8.0, None, ALU.mult), reads=[mask_b], writes=[maskb_b])
        S.op("dve", lambda e: e.tensor_scalar(maskb1[:], mask1[:], 8.0, None, ALU.mult), reads=[mask_b], writes=[maskb_b])
        S.op("dve", lambda e: e.tensor_scalar(sinks8[:], sinks[:], 8.0, None, ALU.mult), reads=[sinks_b], writes=[sinks_b])
        S.op("dve", lambda e: e.tensor_copy(posf[:], posi[:]), reads=[pos_b], writes=[pos_b])
        ang3 = ang.rearrange("p (t j) -> p t j", t=17)
        for t in range(17):
            S.op("dve", lambda e, t=t: e.tensor_scalar(ang3[:, t, :], invf[:], posf[:, t:t + 1], None, ALU.mult),
                 reads=[pos_b, invf_b], writes=[ang_b])

        def sincos(dst, shift):
            S.op("dve", lambda e: e.tensor_scalar(ang2[:], ang[:], shift, 1.0 / TWO_PI, ALU.add, ALU.mult),
                 reads=[ang_b], writes=[rt_b])
            S.op("dve", lambda e: e.tensor_copy(angi[:], ang2[:]), reads=[rt_b], writes=[rt_b])
            S.op("dve", lambda e: e.tensor_copy(ang2[:], angi[:]), reads=[rt_b], writes=[rt_b])
            S.op("dve", lambda e: e.scalar_tensor_tensor(ang2[:], ang2[:], -TWO_PI, ang[:], ALU.mult, ALU.add),
                 reads=[rt_b, ang_b], writes=[rt_b])
            S.op("dve", lambda e: e.tensor_scalar(ang2[:], ang2[:], shift, 3.14159, ALU.add, ALU.min),
                 reads=[rt_b], writes=[rt_b])
            S.op("dve", lambda e: e.tensor_scalar(ang2[:], ang2[:], -3.14159, None, ALU.max),
                 reads=[rt_b], writes=[rt_b])
            S.op("act", lambda e: e.activation(dst.rearrange("p t j -> p (t j)"), ang2[:], AF.Sin),
                 reads=[rt_b], writes=[rope_b])

        sincos(sin_t, 0.0)
        sincos(cos_t, 1.5707963267948966)
        S.op("dve", lambda e: e.tensor_scalar(nsin_t[:, :, :], sin_t[:, :, :], -1.0, None, ALU.mult),
             reads=[rope_b], writes=[rope_b])
        S.barrier()


        def finish_dbg():
            for ot_ in range(NT):
                S.dma("sp", out_d[ot_ * P:(ot_ + 1) * P, :], X[:, ot_, :], reads=[Xb[ot_]])
            S.emit()

        if dbg == "c0":
            finish_dbg()
            return

        def rstd_from_ss(ss_ap, dst_ap, n, rb, wb):
            S.op("act", lambda e: e.activation(dst_ap, ss_ap, AF.Ln, scale=1.0 / n, bias=eps_ap),
                 reads=[rb, eps_b], writes=[wb])
            S.op("act", lambda e: e.activation(dst_ap, dst_ap, AF.Exp, scale=-0.5), reads=[wb], writes=[wb])

        epst = sb("epst", [P, 1]); eps_b = B("eps")
        eps_ap = epst[:, 0:1]
        S.op("dve", lambda e: e.memset(epst[:], EPS), writes=[eps_b])

        psT0 = ps[0][:, :].bitcast(BF16)
        psT1 = ps[1][:, :].bitcast(BF16)
        fm = [ps[2], ps[3], ps[6], ps[7]]
        fm_b = [pb[2], pb[3], pb[6], pb[7]]
        fmi = [0]

        def nextfm():
            i = fmi[0] % 4
            fmi[0] += 1
            return fm[i], fm_b[i]

        hbfs = [hbf, hbf2]; hbf_bs = [hbf_b, hbf2_b]
        scrs = [scr16, scr16]; scr_bs = [scr_b, scr_b]
        smn = [smn0, smn1]; smn_b = [smn0_b, smn1_b]

        def norm1(xap, xbuf, tcol):
            z = tcol % 2
            hb = hbfs[z]; hb_b = hbf_bs[z]; sc = scrs[z]; sc_b = scr_bs[z]; smz = smn[z]; smz_b = smn_b[z]
            S.op("act", lambda e: e.activation(sc[:], xap, AF.Square, accum_out=smz[:, 0:1]),
                 reads=[xbuf], writes=[sc_b, smz_b])
            rstd_from_ss(smz[:, 0:1], smz[:, 1:2], float(D), smz_b, smz_b)
            S.op("dve", lambda e: e.scalar_tensor_tensor(hb[:], xap, smz[:, 1:2], gbc[:], ALU.mult, ALU.mult),
                 reads=[xbuf, smz_b, gbc_b], writes=[hb_b])

        def norm2(tcol):
            z = tcol % 2
            hb = hbfs[z]; hb_b = hbf_bs[z]
            psTz = ps[z][:, :].bitcast(BF16)

            def tr(e):
                ins = None
                for c in range(8):
                    ins = e.transpose(psTz[:, c * P:(c + 1) * P], hb[:, c * P:(c + 1) * P], id16[:])
                return ins
            S.op("pe", tr, reads=[hb_b, id16_b], writes=[pb[z]])
            S.op("act", lambda e: e.copy(hT[:, :, tcol * P:(tcol + 1) * P],
                                         psTz.rearrange("p (c t) -> p c t", c=8)),
                 reads=[pb[z]], writes=[hT_b])

        def norm_transpose_tile(xap, xbuf, tcol):
            norm1(xap, xbuf, tcol)
            norm2(tcol)

        hls = [hl, hl2]; hl_bs = [hl_b, hl2_b]
        FMB = [3, 6, 7, 4, 5, 1]
        LB = [2, 0]
        lbk = [0]

        def nextL():
            i = LB[lbk[0] % 2]
            lbk[0] += 1
            return ps[i], pb[i]
        fmk = [0]

        def nextbank():
            i = FMB[fmk[0] % len(FMB)]
            fmk[0] += 1
            return ps[i], pb[i]

        def lru_block(blk, own):
            t0, t1, t2, t3, t4, t5, t6 = tmp
            b0, b1, b2, b3, b4, b5, b6 = tmp_b
            gates = {}

            def stageA(cc):
                cv = cvs[cc % 3]; cv_b = cvs_b[cc % 3]; cvb = cvbs[cc % 3]; cvb_b = cvbs_b[cc % 3]
                pl, plb = nextL()

                def mm(e, cc=cc, pl=pl):
                    ins = None
                    for c in range(8):
                        ins = e.matmul(pl[:, :], lhsT=Win[:, c, OFF_LX + cc * P:OFF_LX + (cc + 1) * P],
                                       rhs=hT[:, c, :], start=(c == 0), stop=(c == 7))
                    return ins
                S.op("pe", mm, reads=[Win_b, hT_b], writes=[plb])
                if blk == 0:
                    S.op("dve", lambda e, cc=cc: e.memset(lxb[:, cc, 0:3], 0.0), writes=[lxb_b[cc]])
                else:
                    S.op("dve", lambda e, cc=cc: e.tensor_copy(lxb[:, cc, 0:3], lxb[:, cc, 512:515]),
                         reads=[lxb_b[cc]], writes=[lxb_b[cc]])
                S.op("act", lambda e, cc=cc, pl=pl: e.copy(lxb[:, cc, 3:515], pl[:, :]),
                     reads=[plb], writes=[lxb_b[cc]])
                S.op("dve", lambda e, cc=cc, cv=cv: e.tensor_scalar(cv[:], lxb[:, cc, 0:512], colp[:, cc, 0:1],
                                                                     colp[:, cc, 4:5], ALU.mult, ALU.add),
                     reads=[lxb_b[cc], colp_b], writes=[cv_b])
                for j in range(1, 4):
                    S.op("dve", lambda e, cc=cc, j=j, cv=cv: e.scalar_tensor_tensor(
                        cv[:], lxb[:, cc, j:j + 512], colp[:, cc, j:j + 1], cv[:], ALU.mult, ALU.add),
                        reads=[lxb_b[cc], colp_b, cv_b], writes=[cv_b])
                S.op("act", lambda e, cv=cv, cvb=cvb: e.copy(cvb[:], cv[:]), reads=[cv_b], writes=[cvb_b])
                pga, pgab = nextbank()
                S.op("pe", lambda e, cc=cc, pga=pga, cvb=cvb: e.matmul(pga[:, :], lhsT=wabd[:, cc, :], rhs=cvb[:],
                                                                        start=True, stop=True),
                     reads=[wbd_b, cvb_b], writes=[pgab])
                pgx, pgxb = nextbank()
                S.op("pe", lambda e, cc=cc, pgx=pgx, cvb=cvb: e.matmul(pgx[:, :], lhsT=wxbd[:, cc, :], rhs=cvb[:],
                                                                        start=True, stop=True),
                     reads=[wbd_b, cvb_b], writes=[pgxb])
                gates[cc] = (pga, pgab, pgx, pgxb)

            def stageB(cc):
                cv = cvs[cc % 3]; cv_b = cvs_b[cc % 3]
                hlc = hls[cc % 2]; hlc_b = hl_bs[cc % 2]
                pga, pgab, pgx, pgxb = gates[cc][:4]
                t2 = [tmp[2], t2x][cc % 2]; b2 = [tmp_b[2], t2x_b][cc % 2]
                t3 = [tmp[3], t3x][cc % 2]; b3 = [tmp_b[3], t3x_b][cc % 2]
                S.op("act", lambda e, cc=cc, pga=pga: e.activation(t0[:], pga[:, :], AF.Exp, scale=-1.0,
                                                                    bias=der[:, cc, 2:3]),
                     reads=[pgab, der_b], writes=[b0])
                S.op("act", lambda e, cc=cc, pgx=pgx: e.activation(t4[:], pgx[:, :], AF.Exp, scale=-1.0,
                                                                    bias=der[:, cc, 3:4]),
                     reads=[pgxb, der_b], writes=[b4])
                S.op("act", lambda e: e.activation(t0[:], t0[:], AF.Ln, bias=1.0), reads=[b0], writes=[b0])
                S.op("act", lambda e: e.activation(t1[:], t0[:], AF.Exp, scale=-1.0), reads=[b0], writes=[b1])
                S.op("act", lambda e, cc=cc: e.activation(t2[:], t1[:], AF.Exp, scale=der[:, cc, 0:1]),
                     reads=[b1, der_b], writes=[b2])
                S.op("act", lambda e, cc=cc: e.activation(t3[:], t1[:], AF.Exp, scale=der[:, cc, 1:2]),
                     reads=[b1, der_b], writes=[b3])
                S.op("act", lambda e: e.activation(t3[:], t3[:], AF.Ln, scale=-1.0, bias=1.0),
                     reads=[b3], writes=[b3])
                S.op("act", lambda e: e.activation(t4[:], t4[:], AF.Ln, bias=1.0), reads=[b4], writes=[b4])
                S.op("dve", lambda e: e.scalar_tensor_tensor(t3[:], t3[:], 0.5, t4[:], ALU.mult, ALU.subtract),
                     reads=[b3, b4], writes=[b3])
                S.op("act", lambda e: e.activation(t3[:], t3[:], AF.Exp), reads=[b3], writes=[b3])
                S.op("dve", lambda e, cv=cv: e.tensor_tensor(t3[:], t3[:], cv[:], ALU.mult), reads=[b3, cv_b], writes=[b3])
                if blk == 0:
                    S.op("dve", lambda e, cc=cc: e.memset(state[:, cc:cc + 1], 0.0), writes=[state_b])
                if blk == 4:
                    S.op("dve", lambda e, cc=cc: e.tensor_tensor(state[:, cc:cc + 1], state[:, cc:cc + 1],
                                                                  hasp[:, 0:1], ALU.mult),
                         reads=[state_b, hasp_b], writes=[state_b])
                S.op("dve", lambda e, cc=cc, hlc=hlc: e.tensor_tensor_scan(hlc[:], t2[:], t3[:], state[:, cc:cc + 1],
                                                                            ALU.mult, ALU.add),
                     reads=[b2, b3, state_b], writes=[hlc_b])
                S.op("dve", lambda e, cc=cc, hlc=hlc: e.tensor_copy(state[:, cc:cc + 1], hlc[:, 511:512]),
                     reads=[hlc_b], writes=[state_b])

            def stageC(cc):
                hlc = hls[cc % 2]; hlc_b = hl_bs[cc % 2]
                plg, plgb = nextL()

                def mm2(e, cc=cc, plg=plg):
                    ins = None
                    for c in range(8):
                        ins = e.matmul(plg[:, :], lhsT=Win[:, c, OFF_LG + cc * P:OFF_LG + (cc + 1) * P],
                                       rhs=hT[:, c, :], start=(c == 0), stop=(c == 7))
                    return ins
                S.op("pe", mm2, reads=[Win_b, hT_b], writes=[plgb])
                S.op("act", lambda e, plg=plg: e.copy(t5[:], plg[:, :]), reads=[plgb], writes=[b5])
                S.op("dve", lambda e: e.tensor_tensor(t6[:], t5[:], t5[:], ALU.mult), reads=[b5], writes=[b6])
                S.op("dve", lambda e: e.tensor_scalar(t6[:], t6[:], 0.044715, 1.0, ALU.mult, ALU.add),
                     reads=[b6], writes=[b6])
                S.op("dve", lambda e: e.tensor_tensor(t6[:], t6[:], t5[:], ALU.mult), reads=[b6, b5], writes=[b6])
                S.op("act", lambda e: e.activation(t6[:], t6[:], AF.Exp, scale=-GELU_C), reads=[b6], writes=[b6])
                S.op("act", lambda e: e.activation(t6[:], t6[:], AF.Ln, bias=1.0), reads=[b6], writes=[b6])
                S.op("act", lambda e: e.activation(t6[:], t6[:], AF.Exp, scale=-1.0), reads=[b6], writes=[b6])
                S.op("dve", lambda e, hlc=hlc: e.tensor_tensor(t5[:], t5[:], hlc[:], ALU.mult), reads=[b5, hlc_b], writes=[b5])
                S.op("dve", lambda e: e.tensor_tensor(t5[:], t5[:], t6[:], ALU.mult), reads=[b5, b6], writes=[b5])
                S.op("act", lambda e, cc=cc: e.activation(sqT[:, cc, :], t5[:], AF.Square), reads=[b5], writes=[sqT_b])
                S.op("act", lambda e, cc=cc: e.copy(yT[:, cc, :], t5[:]), reads=[b5], writes=[yT_b])

            stageA(0)
            stageA(1)
            stageA(2)
            stageB(0)
            stageA(3)
            stageB(1)
            if own:
                stageC(0)
            stageB(2)
            if own:
                stageC(1)
            stageB(3)
            if own:
                stageC(2)
                stageC(3)
            if own:
                pss, pssb = nextL()

                def mmss(e, pss=pss):
                    ins = None
                    for t in range(4):
                        for cc in range(4):
                            ins = e.matmul(pss[:, t:t + 1], lhsT=sqT[:, cc, t * P:(t + 1) * P], rhs=ones16[:, 0:1],
                                           start=(cc == 0), stop=(cc == 3))
                    return ins
                S.op("pe", mmss, reads=[sqT_b, ones_b], writes=[pssb])
                S.op("act", lambda e, pss=pss: e.copy(sslru[:], pss[:, 0:4]), reads=[pssb], writes=[sslru_b])
                rstd_from_ss(sslru[:], sslru[:], 512.0, sslru_b, sslru_b)

        def kv_tile1(gt, tcol, do_q):
            ti = gt - 15
            slot = gt % 3
            if do_q:
                def mmq(e):
                    ins = None
                    for c in range(8):
                        ins = e.matmul(ps[4][:, :], lhsT=hT[:, c, tcol * P:(tcol + 1) * P], rhs=Win[:, c, 0:512],
                                       start=(c == 0), stop=(c == 7))
                    return ins
                S.op("pe", mmq, reads=[hT_b, Win_b], writes=[pb[4]])
                S.op("act", lambda e: e.copy(qk_sb[:, 0:512], ps[4][:, :]), reads=[pb[4]], writes=[qk_b])

            def mmkv(e):
                ins = None
                for c in range(8):
                    ins = e.matmul(ps[5][:, 0:256], lhsT=hT[:, c, tcol * P:(tcol + 1) * P], rhs=Win[:, c, 512:768],
                                   start=(c == 0), stop=(c == 7))
                return ins
            S.op("pe", mmkv, reads=[hT_b, Win_b], writes=[pb[5]])
            S.op("act", lambda e: e.copy(qk_sb[:, 512:640], ps[5][:, 0:128]), reads=[pb[5]], writes=[qk_b])
            S.op("act", lambda e: e.copy(vv[slot][:], ps[5][:, 128:256]), reads=[pb[5]], writes=[vv_b[slot]])

            h0 = 0 if do_q else 8
            nh = 10 - h0
            v4 = lambda ap: ap.rearrange("p (h two j) -> p h two j", h=10, two=2)[:, h0:10, :, :]
            src4 = v4(qk_sb); A4 = v4(rA); B4 = v4(rB); dst4 = v4(qkr)
            cb = cos_t[:, ti, :].unsqueeze(1).unsqueeze(1).to_broadcast([P, nh, 2, 32])
            sb_ = sin_t[:, ti, :].unsqueeze(1).to_broadcast([P, nh, 32])
            nsb = nsin_t[:, ti, :].unsqueeze(1).to_broadcast([P, nh, 32])
            S.op("dve", lambda e: e.tensor_tensor(A4, src4, cb, ALU.mult), reads=[qk_b, rope_b], writes=[rt_b])
            S.op("dve", lambda e: e.tensor_tensor(B4[:, :, 0, :], src4[:, :, 1, :], nsb, ALU.mult),
                 reads=[qk_b, rope_b], writes=[rt_b])
            S.op("dve", lambda e: e.tensor_tensor(B4[:, :, 1, :], src4[:, :, 0, :], sb_, ALU.mult),
                 reads=[qk_b, rope_b], writes=[rt_b])
            S.op("dve", lambda e: e.tensor_tensor(dst4, A4, B4, ALU.add), reads=[rt_b], writes=[qkr_b])

        def kv_tile2(gt, tcol, do_q):
            slot = gt % 3

            def trk(e):
                ins = None
                for kvh in range(2):
                    ins = e.transpose(psT1[0:64, kvh * P:(kvh + 1) * P], qkr[:, (8 + kvh) * 64:(9 + kvh) * 64], id16[:])
                return ins
            S.op("pe", trk, reads=[qkr_b, id16_b], writes=[pb[1]])
            S.op("act", lambda e: e.copy(kT[slot][0:64, :, :], psT1[0:64, 0:2 * P].rearrange("p (c t) -> p c t", c=2)),
                 reads=[pb[1]], writes=[kT_b[slot]])
            if do_q:
                def trq(e):
                    ins = None
                    for h in range(8):
                        ins = e.transpose(psT0[0:64, h * P:(h + 1) * P], qkr[:, h * 64:(h + 1) * 64], id16[:])
                    return ins
                S.op("pe", trq, reads=[qkr_b, id16_b], writes=[pb[0]])
                S.op("act", lambda e: e.copy(qT[0:64, :, :], psT0[0:64, :].rearrange("p (c t) -> p c t", c=8)),
                     reads=[pb[0]], writes=[qT_b])

        def ck(name):
            if dbg == name:
                raise StopBuild()

        def attn_tile(gt, tcol):
            ot = gt - 16
            slot = gt % 3
            pslot = (gt - 1) % 3
            maskb = maskb0 if ot == 0 else maskb1
            def hp_ctx(hp):
                z = hp % 2
                return dict(kvh=hp // 2, z=z, p_sb=p_sbs[z], p_b=p_bs[z], pT=pTs[z], pT_b=pT_bs[z],
                            sbank=[6, 2][z], tbank=[1, 3][z], psS=ps[[6, 2][z]],
                            psTb=ps[[1, 3][z]][:, :].bitcast(BF16), smk=[sm, sm2][z], smk_b=[sm_b, sm2_b][z])

            def st_sc(hp):
                c = hp_ctx(hp)
                kvh, psS = c["kvh"], c["psS"]

                def mms(e):
                    ins = None
                    for hh in range(2):
                        h = hp * 2 + hh
                        ins = e.matmul(psS[:, hh * 256:(hh + 1) * 256], lhsT=id16[:, :], rhs=maskb[:, :],
                                       start=True, stop=False)
                        ins = e.matmul(psS[:, hh * 256:hh * 256 + 128], lhsT=qT[0:64, h, :],
                                       rhs=kT[pslot][0:64, kvh, :], start=False, stop=False)
                        ins = e.matmul(psS[:, hh * 256 + 128:hh * 256 + 256], lhsT=qT[0:64, h, :],
                                       rhs=kT[slot][0:64, kvh, :], start=False, stop=True)
                    return ins
                S.op("pe", mms, reads=[qT_b, kT_b[0], kT_b[1], kT_b[2], id16_b, maskb_b], writes=[pb[c["sbank"]]])

            def st_smx(hp):
                c = hp_ctx(hp)
                psS, smk, smk_b, p_sb, p_b, sbank = c["psS"], c["smk"], c["smk_b"], c["p_sb"], c["p_b"], c["sbank"]
                mx = smk[:, 8:10]; nb = smk[:, 10:12]; es = smk[:, 12:14]; rs = smk[:, 16:18]
                rinv = rinv8t[:, hp * 2:hp * 2 + 2]
                S.op("dve", lambda e: e.tensor_reduce(mx, psS[:, :].rearrange("p (h k) -> p h k", h=2), AX.X, ALU.max),
                     reads=[pb[sbank]], writes=[smk_b])
                S.op("dve", lambda e: e.tensor_tensor(mx, mx, sinks8[:, hp * 2:hp * 2 + 2], ALU.max),
                     reads=[smk_b, sinks_b], writes=[smk_b])
                S.op("dve", lambda e: e.tensor_scalar(nb, mx, -0.125, None, ALU.mult), reads=[smk_b], writes=[smk_b])
                for hh in range(2):
                    S.op("act", lambda e, hh=hh: e.activation(
                        p_sb[:, hh, :], psS[:, hh * 256:(hh + 1) * 256], AF.Exp, scale=0.125, bias=nb[:, hh:hh + 1],
                        accum_out=rs[:, hh:hh + 1]),
                        reads=[pb[sbank], smk_b], writes=[p_b, smk_b])

            def st_smb(hp):
                c = hp_ctx(hp)
                smk, smk_b = c["smk"], c["smk_b"]
                nb = smk[:, 10:12]; es = smk[:, 12:14]; rs = smk[:, 16:18]
                rinv = rinv8t[:, hp * 2:hp * 2 + 2]
                S.op("dve", lambda e: e.tensor_tensor(es, sinks[:, hp * 2:hp * 2 + 2], nb, ALU.add),
                     reads=[smk_b, sinks_b], writes=[smk_b])
                S.op("act", lambda e: e.activation(es, es, AF.Exp), reads=[smk_b], writes=[smk_b])
                S.op("dve", lambda e: e.tensor_tensor(rs, rs, es, ALU.add), reads=[smk_b], writes=[smk_b])
                S.op("dve", lambda e: e.reciprocal(rinv, rs), reads=[smk_b], writes=[rinv_b])

            def st_tp(hp):
                c = hp_ctx(hp)
                p_sb, p_b, pT, pT_b, psTb, tbank = c["p_sb"], c["p_b"], c["pT"], c["pT_b"], c["psTb"], c["tbank"]

                def trp(e):
                    ins = None
                    for hh in range(2):
                        for kb in range(2):
                            ins = e.transpose(psTb[:, (hh * 2 + kb) * P:(hh * 2 + kb + 1) * P],
                                              p_sb[:, hh, kb * P:(kb + 1) * P], id16[:])
                    return ins
                S.op("pe", trp, reads=[p_b, id16_b], writes=[pb[tbank]])
                S.op("act", lambda e: e.copy(pT[:, :, :], psTb[:, 0:4 * P].rearrange("p (h t) -> p h t", h=4)),
                     reads=[pb[tbank]], writes=[pT_b])

            def st_pv(hp):
                c = hp_ctx(hp)
                kvh, pT, pT_b = c["kvh"], c["pT"], c["pT_b"]

                def mmo(e):
                    ins = None
                    for hh in range(2):
                        h = hp * 2 + hh
                        ins = e.matmul(ps[7][:, h * 64:(h + 1) * 64], lhsT=pT[:, hh * 2, :],
                                       rhs=vv[pslot][:, kvh * 64:(kvh + 1) * 64], start=True, stop=False)
                        ins = e.matmul(ps[7][:, h * 64:(h + 1) * 64], lhsT=pT[:, hh * 2 + 1, :],
                                       rhs=vv[slot][:, kvh * 64:(kvh + 1) * 64], start=False, stop=True)
                    return ins
                S.op("pe", mmo, reads=[pT_b, vv_b[0], vv_b[1], vv_b[2]], writes=[pb[7]])

            st_sc(0); st_sc(1); st_smx(0); st_tp(0); st_sc(2); st_smx(1); st_smb(0); st_pv(0); st_tp(1)
            st_sc(3); st_smx(2); st_smb(1); st_pv(1); st_tp(2); st_smx(3); st_smb(2); st_pv(2); st_tp(3); st_smb(3); st_pv(3)
            ck("A7")
            rinv8 = rinv8t[:, 0:8]
            S.op("dve", lambda e: e.tensor_tensor(attn.rearrange("p (h d) -> p h d", h=8),
                                                  ps[7][:, :].rearrange("p (h d) -> p h d", h=8),
                                                  rinv8.unsqueeze(2).to_broadcast([P, 8, 64]), ALU.mult),
                 reads=[pb[7], rinv_b], writes=[attn_b])
            ck("A8")
            S.op("act", lambda e: e.activation(scr16[:, 0:512], attn[:], AF.Square, accum_out=sm[:, 2:3]),
                 reads=[attn_b], writes=[scr_b, sm_b])
            rstd_from_ss(sm[:, 2:3], sm[:, 3:4], 512.0, sm_b, sm_b)
            S.op("dve", lambda e: e.tensor_scalar(attnb[:], attn[:], sm[:, 3:4], None, ALU.mult),
                 reads=[attn_b, sm_b], writes=[attnb_b])

            def tra(e):
                ins = None
                for c in range(4):
                    ins = e.transpose(psT0[:, c * P:(c + 1) * P], attnb[:, c * P:(c + 1) * P], id16[:])
                return ins
            S.op("pe", tra, reads=[attnb_b, id16_b], writes=[pb[0]])
            S.op("act", lambda e: e.copy(attnT[:, :, :], psT0[:, 0:4 * P].rearrange("p (c t) -> p c t", c=4)),
                 reads=[pb[0]], writes=[attnT_b])
            ck("A9")
            def mmoa(e):
                ins = None
                for dh in range(2):
                    for c in range(4):
                        ins = e.matmul(ps[4 + dh][:, :], lhsT=attnT[:, c, :], rhs=Wout[:, c, dh * 512:(dh + 1) * 512],
                                       start=(c == 0), stop=(c == 3))
                return ins
            S.op("pe", mmoa, reads=[attnT_b, Wout_b], writes=[pb[4], pb[5]])
            for dh in range(2):
                S.op("dve", lambda e, dh=dh: e.tensor_tensor(X[:, ot, dh * 512:(dh + 1) * 512],
                                                             X[:, ot, dh * 512:(dh + 1) * 512], ps[4 + dh][:, :], ALU.add),
                     reads=[pb[4 + dh], Xb[ot]], writes=[Xb[ot]])

            def mmol(e):
                ins = None
                for dh in range(2):
                    for c in range(4):
                        ins = e.matmul(ps[4 + dh][:, :], lhsT=yT[:, c, tcol * P:(tcol + 1) * P],
                                       rhs=Wout[:, 4 + c, dh * 512:(dh + 1) * 512], start=(c == 0), stop=(c == 3))
                return ins
            S.op("pe", mmol, reads=[yT_b, Wout_b], writes=[pb[4], pb[5]])
            for dh in range(2):
                S.op("dve", lambda e, dh=dh: e.scalar_tensor_tensor(
                    X[:, ot, dh * 512:(dh + 1) * 512], ps[4 + dh][:, :], sslru[:, tcol:tcol + 1],
                    X[:, ot, dh * 512:(dh + 1) * 512], ALU.mult, ALU.add),
                    reads=[pb[4 + dh], Xb[ot], sslru_b], writes=[Xb[ot]])

        for blk in range(8):
            own = blk >= 4
            if blk == 2:
                for c in range(8):
                    S.dma("pool", Wout[:, c, :], wout_d[c * P:(c + 1) * P, :], writes=[Wout_b])
            if blk == 3:
                for c in range(8):
                    gcol = colp[:, c, 9:10] if c < 4 else colp[:, c - 4, 8:9]
                    S.op("dve", lambda e, c=c, gcol=gcol: e.tensor_scalar(Wout[:, c, :], Wout[:, c, :], gcol, None, ALU.mult),
                         reads=[Wout_b, colp_b], writes=[Wout_b])
            if dbg and dbg[0] == "A" and blk == 4:
                try:
                    S.dma("sp", X[:, 0, :], x_d[16 * P:17 * P, :], writes=[Xb[0]])
                    norm_transpose_tile(X[:, 0, :], Xb[0], 0)
                    lru_block(4, True)
                    kv_tile1(16, 0, True)
                    kv_tile2(16, 0, True)
                    attn_tile(16, 0)
                except StopBuild:
                    pass
                finish_dbg()
                return
            xi = [(blk * 4 + t) - (16 if own else 0) for t in range(4)]
            for t in range(4):
                S.dma("sp", X[:, xi[t], :], x_d[(blk * 4 + t) * P:(blk * 4 + t + 1) * P, :], writes=[Xb[xi[t]]])
            norm1(X[:, xi[0], :], Xb[xi[0]], 0)
            norm1(X[:, xi[1], :], Xb[xi[1]], 1)
            norm2(0)
            norm1(X[:, xi[2], :], Xb[xi[2]], 2)
            norm2(1)
            norm1(X[:, xi[3], :], Xb[xi[3]], 3)
            norm2(2)
            norm2(3)
            if dbg == "n%d" % blk:
                finish_dbg()
                return
            lru_block(blk, own)
            if dbg == "b%d" % blk:
                finish_dbg()
                return
            if blk == 3:
                kv_tile1(15, 3, False)
                kv_tile2(15, 3, False)
            if dbg == "k%d" % blk:
                finish_dbg()
                return
            if own:
                g0_ = blk * 4
                kv_tile1(g0_, 0, True)
                kv_tile2(g0_, 0, True)
                for t in range(4):
                    gt = g0_ + t
                    if t < 3:
                        kv_tile1(gt + 1, t + 1, True)
                    attn_tile(gt, t)
                    if t < 3:
                        kv_tile2(gt + 1, t + 1, True)

        if dbg == "p1":
            for ot in range(NT):
                S.dma("sp", out_d[ot * P:(ot + 1) * P, :], X[:, ot, :], reads=[Xb[ot]])
            S.emit()
            return

        S.barrier()
        R.off = 0
        h2T = R.alloc(8 * T_OWN, BF16).rearrange("p (c t) -> p c t", c=8); h2T_b = B("h2T")
        cw = R.alloc(NT * 32).rearrange("p (t e) -> p t e", t=NT); cw_b = B("cw")
        wgs = [R.alloc(8 * 512, BF16).rearrange("p (c f) -> p c f", c=8) for _ in range(2)]
        wus = [R.alloc(8 * 512, BF16).rearrange("p (c f) -> p c f", c=8) for _ in range(2)]
        wds = [R.alloc(4 * D, BF16).rearrange("p (c d) -> p c d", c=4) for _ in range(2)]
        wsl_b = [B("wslot0"), B("wslot1")]
        sgs = [R.alloc(512) for _ in range(2)]; sg_b = [B("sg0"), B("sg1")]
        hid = [R.alloc(4 * 512, BF16).rearrange("p (c t) -> p c t", c=4) for _ in range(2)]
        hid_b = [B("hid0"), B("hid1")]
        mark = R.off
        nE = NE if dbg != "fewexp" else 2
        for E in range(min(2, nE)):
            S.dma("pool", wgs[E][:], wg_d[E].rearrange("(c p) f -> p c f", p=P), writes=[wsl_b[E]])
            S.dma("pool", wus[E][:], wu_d[E].rearrange("(c p) f -> p c f", p=P), writes=[wsl_b[E]])
            S.dma("pool", wds[E][:], wd_d[E].rearrange("(c p) d -> p c d", p=P), writes=[wsl_b[E]])
        wrt = R.alloc(8 * 36).rearrange("p (c n) -> p c n", c=8); wrt_b = B("wrt")
        brt = R.alloc(36); brt_b = B("brt")
        h2s = [R.alloc(D) for _ in range(2)]; h2_bs = [B("h2a"), B("h2b")]
        h2T32s = [R.alloc(8 * P).rearrange("p (c t) -> p c t", c=8) for _ in range(2)]; h2T32_bs = [B("h2Ta"), B("h2Tb")]
        small2 = R.alloc(16); small2_b = B("small2")
        sq2 = R.alloc(D, BF16); sq2_b = B("sq2")
        lg = R.alloc(NT * 36).rearrange("p (t n) -> p t n", t=NT); lg_b = B("lg")
        rt1 = R.alloc(NT * 32); rt2 = R.alloc(NT * 32); rt3 = R.alloc(NT * 32)
        rtb = B("rtmp")
        S.dma("sp", gbc[:], gains_d[1:2, :].partition_broadcast(P), writes=[gbc_b])
        S.dma("sp", wrt[:], wrt_d.rearrange("(c p) n -> p c n", p=P), writes=[wrt_b])
        S.dma("sp", brt[:], brt_d.partition_broadcast(P), writes=[brt_b])
        ssq = R.alloc(16); ssq_b = B("ssq")
        for ot in range(NT):
            sq = scr16 if ot % 2 == 0 else sq2
            sq_b = scr_b if ot % 2 == 0 else sq2_b
            S.op("act", lambda e, ot=ot, sq=sq: e.activation(sq[:], X[:, ot, :], AF.Square, accum_out=ssq[:, ot:ot + 1]),
                 reads=[Xb[ot]], writes=[sq_b, ssq_b])
        rstd_from_ss(ssq[:], ssq[:], float(D), ssq_b, ssq_b)

        def q1(ot):
            z = ot % 2
            h2 = h2s[z]; h2_b = h2_bs[z]
            S.op("dve", lambda e: e.scalar_tensor_tensor(h2[:], X[:, ot, :], ssq[:, ot:ot + 1], gbc[:], ALU.mult, ALU.mult),
                 reads=[Xb[ot], ssq_b, gbc_b], writes=[h2_b])

        def q2(ot):
            z = ot % 2
            h2 = h2s[z]; h2_b = h2_bs[z]; h2T32 = h2T32s[z]; h2T32_b = h2T32_bs[z]
            for hf in range(2):
                bk = z * 2 + hf

                def trf(e, hf=hf, bk=bk):
                    ins = None
                    for c in range(4):
                        cg = hf * 4 + c
                        ins = e.transpose(ps[bk][:, c * P:(c + 1) * P], h2[:, cg * P:(cg + 1) * P], id32[:])
                    return ins
                S.op("pe", trf, reads=[h2_b, id32_b], writes=[pb[bk]])
                S.op("act", lambda e, hf=hf, bk=bk: e.copy(h2T32[:, hf * 4:hf * 4 + 4, :],
                                                           ps[bk][:, :].rearrange("p (c t) -> p c t", c=4)),
                     reads=[pb[bk]], writes=[h2T32_b])
            S.op("act", lambda e: e.copy(h2T[:, :, ot * P:(ot + 1) * P], h2T32[:, :, :]),
                 reads=[h2T32_b], writes=[h2T_b])

            def mmr(e):
                ins = None
                for c in range(8):
                    ins = e.matmul(ps[4 + z][:, 0:36], lhsT=h2T32[:, c, :], rhs=wrt[:, c, :], start=(c == 0), stop=(c == 7))
                return ins
            S.op("pe", mmr, reads=[h2T32_b, wrt_b], writes=[pb[4 + z]])
            S.op("dve", lambda e: e.tensor_tensor(lg[:, ot, :], ps[4 + z][:, 0:36], brt[:], ALU.add),
                 reads=[pb[4 + z], brt_b], writes=[lg_b])

        q1(0)
        for ot in range(NT):
            if ot + 1 < NT:
                q1(ot + 1)
            q2(ot)

        gl = lg[:, :, 0:4]
        el = lg[:, :, 4:36].rearrange("p t (g e) -> p t g e", g=4)
        gmax = small[:, 8:24]
        gm = rt1[:, 0:64].rearrange("p (t g) -> p t g", t=NT)
        gex = rt1[:, 64:128].rearrange("p (t g) -> p t g", t=NT)
        gtp = small[:, 24:40]
        esel = rt2[:, 0:128].rearrange("p (t e) -> p t e", t=NT)
        etmp = rt2[:, 128:256].rearrange("p (t e) -> p t e", t=NT)
        m1 = rt1[:, 128:144]; m2 = rt1[:, 144:160]; p1 = rt1[:, 160:176]; w1 = rt1[:, 176:192]; w2 = rt1[:, 192:208]
        mk1 = rt3[:, 0:128].rearrange("p (t e) -> p t e", t=NT)
        mk2 = rt3[:, 128:256].rearrange("p (t e) -> p t e", t=NT)
        e2 = rt3[:, 256:384].rearrange("p (t e) -> p t e", t=NT)
        ew = rt2[:, 256:384].rearrange("p (t e) -> p t e", t=NT)
        RB = [lg_b, rtb, small_b]

        def dv(fn):
            S.op("dve", fn, reads=RB, writes=[rtb, small_b])

        bc3 = lambda ap2, n: ap2.unsqueeze(2).to_broadcast([P, NT, n])
        dv(lambda e: e.tensor_reduce(gmax, gl, AX.X, ALU.max))
        dv(lambda e: e.tensor_tensor(gm, gl, bc3(gmax, 4), ALU.is_equal))
        dv(lambda e: e.tensor_tensor(gex, gl, bc3(gmax, 4), ALU.subtract))
        S.op("act", lambda e: e.activation(gex, gex, AF.Exp), reads=[rtb], writes=[rtb])
        dv(lambda e: e.tensor_reduce(gtp, gex, AX.X, ALU.add))
        dv(lambda e: e.reciprocal(gtp, gtp))
        for g in range(4):
            if g == 0:
                dv(lambda e: e.tensor_tensor(esel, el[:, :, 0, :], bc3(gm[:, :, 0], 8), ALU.mult))
            else:
                dv(lambda e, g=g: e.tensor_tensor(etmp, el[:, :, g, :], bc3(gm[:, :, g], 8), ALU.mult))
                dv(lambda e: e.tensor_tensor(esel, esel, etmp, ALU.add))
        dv(lambda e: e.tensor_reduce(m1, esel, AX.X, ALU.max))
        dv(lambda e: e.tensor_tensor(mk1, esel, bc3(m1, 8), ALU.is_equal))
        dv(lambda e: e.scalar_tensor_tensor(e2, mk1, -1.0e30, esel, ALU.mult, ALU.add))
        dv(lambda e: e.tensor_reduce(m2, e2, AX.X, ALU.max))
        dv(lambda e: e.tensor_tensor(mk2, e2, bc3(m2, 8), ALU.is_equal))
        dv(lambda e: e.tensor_tensor(p1, m2, m1, ALU.subtract))
        S.op("act", lambda e: e.activation(p1, p1, AF.Exp), reads=[rtb], writes=[rtb])
        dv(lambda e: e.tensor_scalar(p1, p1, 1.0, None, ALU.add))
        dv(lambda e: e.reciprocal(p1, p1))
        dv(lambda e: e.tensor_tensor(w1, p1, gtp, ALU.mult))
        dv(lambda e: e.tensor_tensor(w2, gtp, w1, ALU.subtract))
        dv(lambda e: e.tensor_tensor(ew, mk1, bc3(w1, 8), ALU.mult))
        dv(lambda e: e.tensor_tensor(etmp, mk2, bc3(w2, 8), ALU.mult))
        dv(lambda e: e.tensor_tensor(ew, ew, etmp, ALU.add))
        cw4 = cw.rearrange("p t (g e) -> p t g e", g=4)
        for g in range(4):
            S.op("dve", lambda e, g=g: e.tensor_tensor(cw4[:, :, g, :], ew, bc3(gm[:, :, g], 8), ALU.mult),
                 reads=[rtb], writes=[cw_b])

        if dbg == "p2":
            S.op("dve", lambda e: e.tensor_copy(X[:, 0, 0:512], cw.rearrange("p t e -> p (t e)")),
                 reads=[cw_b, Xb[0]], writes=[Xb[0]])
            for ot in range(NT):
                S.dma("sp", out_d[ot * P:(ot + 1) * P, :], X[:, ot, :], reads=[Xb[ot]])
            S.emit()
            return

        save_off = R.off
        R.off = mark
        Wpg = R.alloc(8 * D, BF16).rearrange("p (c n) -> p c n", c=8); Wpg_b = B("Wpg")
        Wpp = R.alloc(2 * D, BF16).rearrange("p (c n) -> p c n", c=2); Wpp_b = B("Wpp")
        assert R.off <= save_off
        R.off = save_off
        p2_done = [cw_b, h2T_b, lg_b, rtb, small_b, small2_b, ssq_b, sq2_b, scr_b]
        print("phase3 region words", R.off, "of", RW)
        step = 0
        pending = [None]

        def emit_down(s_, hs, tb, E):
            for t in range(4):
                ot = tb * 4 + t
                for dh in range(2):
                    bank = 4 + (t % 2) * 2 + dh

                    def mmd(e, t=t, dh=dh, bank=bank):
                        ins = None
                        for fc in range(4):
                            ins = e.matmul(ps[bank][:, :], lhsT=hid[hs][:, fc, t * P:(t + 1) * P],
                                           rhs=wds[s_][:, fc, dh * 512:(dh + 1) * 512], start=(fc == 0), stop=(fc == 3))
                        return ins
                    S.op("pe", mmd, reads=[hid_b[hs], wsl_b[s_]], writes=[pb[bank]])
                    S.op("dve", lambda e, ot=ot, dh=dh, bank=bank: e.scalar_tensor_tensor(
                        X[:, ot, dh * 512:(dh + 1) * 512], ps[bank][:, :], cw[:, ot, E:E + 1],
                        X[:, ot, dh * 512:(dh + 1) * 512], ALU.mult, ALU.add),
                        reads=[pb[bank], cw_b, Xb[ot]], writes=[Xb[ot]])

        for E in range(nE):
            s_ = E % 2
            if E == nE - 3 or (nE < 4 and E == 0):
                for c in range(8):
                    S.dma("pool", Wpg[:, c, :], wpg_d[c * P:(c + 1) * P, :], reads=p2_done, writes=[Wpg_b], sembuf=Wpg_b)
                S.dma("pool", Wpp[:], wpp_d.rearrange("(c p) n -> p c n", p=P), reads=p2_done, writes=[Wpp_b], sembuf=Wpp_b)
                S.dma("sp", gbc[:], gains_d[2:3, :].partition_broadcast(P), writes=[gbc_b])
            if E >= 2:
                S.dma("pool", wgs[s_][:], wg_d[E].rearrange("(c p) f -> p c f", p=P), writes=[wsl_b[s_]])
                S.dma("pool", wus[s_][:], wu_d[E].rearrange("(c p) f -> p c f", p=P), writes=[wsl_b[s_]])
                S.dma("pool", wds[s_][:], wd_d[E].rearrange("(c p) d -> p c d", p=P), writes=[wsl_b[s_]])
            for tb in range(4):
                hs = step % 2
                step += 1
                for fc in range(4):
                    gi = fc % 2

                    def mmg(e, s_=s_, fc=fc, tb=tb, gi=gi):
                        ins = None
                        for c in range(8):
                            ins = e.matmul(ps[gi][:, :], lhsT=wgs[s_][:, c, fc * P:(fc + 1) * P],
                                           rhs=h2T[:, c, tb * 512:(tb + 1) * 512], start=(c == 0), stop=(c == 7))
                        return ins
                    S.op("pe", mmg, reads=[wsl_b[s_], h2T_b], writes=[pb[gi]])

                    def mmu(e, s_=s_, fc=fc, tb=tb, gi=gi):
                        ins = None
                        for c in range(8):
                            ins = e.matmul(ps[2 + gi][:, :], lhsT=wus[s_][:, c, fc * P:(fc + 1) * P],
                                           rhs=h2T[:, c, tb * 512:(tb + 1) * 512], start=(c == 0), stop=(c == 7))
                        return ins
                    S.op("pe", mmu, reads=[wsl_b[s_], h2T_b], writes=[pb[2 + gi]])
                    S.op("act", lambda e, gi=gi: e.activation(sgs[gi][:], ps[gi][:, :], AF.Silu),
                         reads=[pb[gi]], writes=[sg_b[gi]])
                    S.op("dve", lambda e, gi=gi, hs=hs, fc=fc: e.tensor_tensor(hid[hs][:, fc, :], sgs[gi][:],
                                                                                  ps[2 + gi][:, :], ALU.mult),
                         reads=[sg_b[gi], pb[2 + gi]], writes=[hid_b[hs]])
                    if fc == 1 and pending[0] is not None:
                        emit_down(*pending[0])
                        pending[0] = None
                pending[0] = (s_, hs, tb, E)
        if pending[0] is not None:
            emit_down(*pending[0])
            pending[0] = None

        S.barrier()
        R.off = 0
        gfin = R.alloc(D); gfin_b = B("gfin")
        h3a = [R.alloc(D, BF16) for _ in range(2)]; h3a_b = [B("h3a0"), B("h3a1")]
        h3T_all = R.alloc(NT * 8 * P, BF16).rearrange("p (t c k) -> p t c k", t=NT, c=8)
        h3T_ab = [B(f"h3T{t}") for t in range(NT)]
        pt32 = [R.alloc(256) for _ in range(2)]; pt32_b = [B("pt0"), B("pt1")]
        pt16s = [R.alloc(256, BF16) for _ in range(2)]; pt16_bs = [B("pt16a"), B("pt16b")]
        ppTs = [R.alloc(2 * P, BF16).rearrange("p (c t) -> p c t", c=2) for _ in range(2)]; ppT_bs = [B("ppTa"), B("ppTb")]
        sgates = [R.alloc(D) for _ in range(2)]; sgate_bs = [B("sga"), B("sgb")]
        sq4 = [R.alloc(D, BF16) for _ in range(2)]; sq4_b = [B("sq4a"), B("sq4b")]
        ob = [R.alloc(D) for _ in range(2)]; ob_b = [B("ob0"), B("ob1")]
        ss1 = R.alloc(16); ss1_b = B("ss1")
        ss2 = R.alloc(16); ss2_b = B("ss2")
        assert R.off <= mark, (R.off, mark)
        S.dma("sp", gfin[:], gains_d[3:4, :].partition_broadcast(P), writes=[gfin_b])
        for ot in range(NT):
            sl = ot % 2
            S.op("act", lambda e, ot=ot, sl=sl: e.activation(sq4[sl][:], X[:, ot, :], AF.Square, accum_out=ss1[:, ot:ot + 1]),
                 reads=[Xb[ot]], writes=[sq4_b[sl], ss1_b])
        rstd_from_ss(ss1[:], ss1[:], float(D), ss1_b, ss1_b)

        def n4(ot):
            z = ot % 2
            S.op("dve", lambda e: e.scalar_tensor_tensor(h3a[z][:], X[:, ot, :], ss1[:, ot:ot + 1], gbc[:], ALU.mult, ALU.mult),
                 reads=[Xb[ot], ss1_b, gbc_b], writes=[h3a_b[z]])

        def t4(ot):
            z = ot % 2
            psTz = ps[z][:, :].bitcast(BF16)

            def tr3(e):
                ins = None
                for cc in range(8):
                    ins = e.transpose(psTz[:, cc * P:(cc + 1) * P], h3a[z][:, cc * P:(cc + 1) * P], id16[:])
                return ins
            S.op("pe", tr3, reads=[h3a_b[z], id16_b], writes=[pb[z]])
            S.op("act", lambda e: e.copy(h3T_all[:, ot, :, :], psTz.rearrange("p (c t) -> p c t", c=8)),
                 reads=[pb[z]], writes=[h3T_ab[ot]])

        n4(0)
        for ot in range(NT):
            if ot + 1 < NT:
                n4(ot + 1)
            t4(ot)

        psTp = ps[0][:, :].bitcast(BF16)

        def pA(ot):
            sl = ot % 2
            S.dma("sp", pt32[sl][:], pp_d[ot * P:(ot + 1) * P, :], writes=[pt32_b[sl]])
            S.op("act", lambda e: e.copy(pt16s[sl][:], pt32[sl][:]), reads=[pt32_b[sl]], writes=[pt16_bs[sl]])

            def trp2(e):
                ins = None
                for cc in range(2):
                    ins = e.transpose(psTp[:, cc * P:(cc + 1) * P], pt16s[sl][:, cc * P:(cc + 1) * P], id16[:])
                return ins
            S.op("pe", trp2, reads=[pt16_bs[sl], id16_b], writes=[pb[0]])
            S.op("act", lambda e: e.copy(ppTs[sl][:, :, :], psTp[:, 0:2 * P].rearrange("p (c t) -> p c t", c=2)),
                 reads=[pb[0]], writes=[ppT_bs[sl]])

        def pG(ot):
            sl = ot % 2
            for dh in range(2):
                gbk = 2 + sl * 2 + dh

                def mmpg(e, dh=dh, gbk=gbk):
                    ins = None
                    for cc in range(8):
                        ins = e.matmul(ps[gbk][:, :], lhsT=h3T_all[:, ot, cc, :], rhs=Wpg[:, cc, dh * 512:(dh + 1) * 512],
                                       start=(cc == 0), stop=(cc == 7))
                    return ins
                S.op("pe", mmpg, reads=[h3T_ab[ot], Wpg_b], writes=[pb[gbk]])

        def pP(ot):
            sl = ot % 2
            for dh in range(2):
                def mmpp(e, dh=dh):
                    ins = None
                    for cc in range(2):
                        ins = e.matmul(ps[6 + dh][:, :], lhsT=ppTs[sl][:, cc, :], rhs=Wpp[:, cc, dh * 512:(dh + 1) * 512],
                                       start=(cc == 0), stop=(cc == 1))
                    return ins
                S.op("pe", mmpp, reads=[ppT_bs[sl], Wpp_b], writes=[pb[6 + dh]])

        def pU(ot):
            sl = ot % 2
            sgate = sgates[sl]; sgate_b = sgate_bs[sl]
            for dh in range(2):
                gbk = 2 + sl * 2 + dh
                sgd = sgate[:, dh * 512:(dh + 1) * 512]
                S.op("act", lambda e, gbk=gbk, sgd=sgd: e.activation(sgd, ps[gbk][:, :], AF.Sigmoid),
                     reads=[pb[gbk]], writes=[sgate_b])
                S.op("dve", lambda e, dh=dh, sgd=sgd: e.tensor_tensor(sgd, sgd, ps[6 + dh][:, :], ALU.mult),
                     reads=[sgate_b, pb[6 + dh]], writes=[sgate_b])
            S.op("dve", lambda e: e.tensor_tensor(X[:, ot, :], X[:, ot, :], sgate[:], ALU.add),
                 reads=[sgate_b, Xb[ot]], writes=[Xb[ot]])
            S.op("act", lambda e: e.activation(sq4[sl][:], X[:, ot, :], AF.Square, accum_out=ss2[:, ot:ot + 1]),
                 reads=[Xb[ot]], writes=[sq4_b[sl], ss2_b])

        pA(0)
        pG(0)
        for ot in range(NT):
            if ot + 1 < NT:
                pA(ot + 1)
            pP(ot)
            if ot + 1 < NT:
                pG(ot + 1)
            pU(ot)
        rstd_from_ss(ss2[:], ss2[:], float(D), ss2_b, ss2_b)
        for ot in range(NT):
            sl = ot % 2
            S.op("dve", lambda e, ot=ot, sl=sl: e.scalar_tensor_tensor(ob[sl][:], X[:, ot, :], ss2[:, ot:ot + 1], gfin[:],
                                                                       ALU.mult, ALU.mult),
                 reads=[Xb[ot], ss2_b, gfin_b], writes=[ob_b[sl]])
            S.dma("sp", out_d[ot * P:(ot + 1) * P, :], ob[sl][:], reads=[ob_b[sl]])
        S.emit()


def make_in_maps(inp, dbg=None):
    f = lambda a: np.ascontiguousarray(np.asarray(a), dtype=np.float32)
    x = f(inp["x"]); p = f(inp["p"])[0]
    positions = np.asarray(inp["positions"]).astype(np.int32)
    w_in = f(inp["w_in"])[0]; w_out = f(inp["w_out"])[0]
    w_rt = np.ascontiguousarray(np.concatenate([f(inp["w_router_group"])[0], f(inp["w_router_expert"])[0]], axis=1))
    b_rt = np.ascontiguousarray(np.concatenate([f(inp["b_router_group"])[0], f(inp["b_router_expert"])[0]], axis=0)[None, :])
    wg = np.ascontiguousarray(f(inp["w_expert_gate"])[0].reshape(NE, D, 512))
    wu = np.ascontiguousarray(f(inp["w_expert_up"])[0].reshape(NE, D, 512))
    wd = np.ascontiguousarray(f(inp["w_expert_down"])[0].reshape(NE, 512, D))
    w_pg = f(inp["w_ple_gate"])[0]; w_pp = f(inp["w_ple_proj"])[0]
    gains = np.ascontiguousarray(np.stack([f(inp["g_mix"])[0], f(inp["g_ffn"])[0], f(inp["g_ple"])[0], f(inp["g_final"])], axis=0))
    cols = [f(inp["conv_w"])[0][j] for j in range(4)] + [f(inp["conv_b"])[0], f(inp["lru_ba"])[0], f(inp["lru_bx"])[0],
                                                          f(inp["lru_lambda"])[0], f(inp["g_lru_out"])[0], f(inp["g_attn_out"])[0]]
    colp = np.ascontiguousarray(np.stack([c.reshape(4, P).T for c in cols], axis=2))
    wa = f(inp["lru_wa"])[0]; wx = f(inp["lru_wx"])[0]
    wa_bd = np.zeros((P, 4, P), np.float32); wx_bd = np.zeros((P, 4, P), np.float32)
    for cc in range(4):
        for hh in range(2):
            wa_bd[hh * 64:(hh + 1) * 64, cc, hh * 64:(hh + 1) * 64] = wa[2 * cc + hh]
            wx_bd[hh * 64:(hh + 1) * 64, cc, hh * 64:(hh + 1) * 64] = wx[2 * cc + hh]
    sinks = f(inp["sinks"])
    qi = np.arange(P)[:, None]; kj = np.arange(P)[None, :]
    maskc = np.where(kj <= qi, 0.0, NEG).astype(np.float32)
    maskp = np.where(kj > qi, 0.0, NEG).astype(np.float32)
    mask1 = np.ascontiguousarray(np.concatenate([maskp, maskc], axis=1))
    mask0_first = np.ascontiguousarray(np.concatenate([np.full((P, P), NEG, np.float32), maskc], axis=1))
    ident = np.eye(P, dtype=np.float32)
    invf = np.ascontiguousarray(np.broadcast_to(
        (10000.0 ** (-np.arange(0, 64, 2, dtype=np.float32) / 64.0)).astype(np.float32)[None, :], (P, 32)))
    if dbg not in (None, "fewexp"):
        wg, wu, wd = wg[:1], wu[:1], wd[:1]
    shared = dict(w_in=w_in, w_out=w_out, w_rt=w_rt, b_rt=b_rt, wg=wg, wu=wu, wd=wd, w_pg=w_pg, w_pp=w_pp,
                  gains=gains, colp=colp, wa_bd=wa_bd, wx_bd=wx_bd, sinks=sinks, mask1=mask1, ident=ident, invf=invf)
    maps = []
    for core in range(8):
        b, h = core // 2, core % 2
        xin = np.zeros((4096, D), np.float32)
        xin[2048:] = x[b, h * 2048:(h + 1) * 2048]
        pos17 = np.zeros((17, P), np.int32)
        pos17[1:] = positions[b, h * 2048:(h + 1) * 2048].reshape(16, P)
        if h == 1:
            xin[:2048] = x[b, 0:2048]
            pos17[0] = positions[b, 2048 - P:2048]
        m = dict(shared)
        m["xin"] = xin
        m["pos"] = np.ascontiguousarray(pos17.T)
        m["pple"] = np.ascontiguousarray(p[b, h * 2048:(h + 1) * 2048])
        m["mask0"] = mask1 if h == 1 else mask0_first
        m["hasprev"] = np.full((P, 1), float(h), np.float32)
        maps.append(m)
    return maps


def kernel(**inputs):
    nc = bass.Bass("TRN2", target_bir_lowering=False)
    build(nc, DEBUG_STOP)
    maps = make_in_maps(inputs)
    res = run_bass_kernel_spmd(nc, maps, core_ids=list(range(8)))
    out = np.zeros((4, 4096, D), np.float32)
    for core in range(8):
        b, h = core // 2, core % 2
        out[b, h * 2048:(h + 1) * 2048] = res.results[core]["out"]
    return out
```

```python
import numpy as np
from contextlib import ExitStack
import concourse.bass as bass
import concourse.mybir as mybir
from concourse.bass_utils import run_bass_kernel_spmd

F32 = mybir.dt.float32
BF16 = mybir.dt.bfloat16
I32 = mybir.dt.int32
AF = mybir.ActivationFunctionType
ALU = mybir.AluOpType
AX = mybir.AxisListType

ENGS = ("pe", "act", "dve", "pool", "sp")


class Buf:
    __slots__ = ("name", "writer", "readers", "dsem", "dcnt")

    def __init__(self, name):
        self.name = name
        self.writer = None
        self.readers = []
        self.dsem = None
        self.dcnt = 0


class Sched:
    def __init__(self, nc, stack):
        self.nc = nc
        self.stack = stack
        self.ops = {e: [] for e in ENGS}
        self.sems = {}
        self.cnt = {}
        for e in ("pe", "act", "dve", "pool"):
            self.sems[e] = stack.enter_context(nc.semaphore("s_" + e))
            self.cnt[e] = 0
        self.seen = {e: {} for e in ENGS}
        self.ndsem = 0
        self.nbuf = 0
        self.dtot = {}

    def buf(self, name=None):
        self.nbuf += 1
        return Buf(name or f"b{self.nbuf}")

    def _dsem(self, b):
        if b.dsem is None:
            self.ndsem += 1
            key = f"d{self.ndsem}"
            self.sems[key] = self.stack.enter_context(self.nc.semaphore("sd%d" % self.ndsem))
            b.dsem = key
            self.dtot[key] = 0
        return b.dsem

    def _collect(self, eng, reads, writes):
        need = {}

        def add(tok):
            if tok is None:
                return
            k, v = tok
            if eng == "pe" and k == "pe":
                return
            if need.get(k, 0) < v:
                need[k] = v

        for b in reads:
            add(b.writer)
        for b in writes:
            add(b.writer)
            for r in b.readers:
                add(r)
        waits = []
        seen = self.seen[eng]
        for k, v in need.items():
            if seen.get(k, 0) < v:
                seen[k] = v
                waits.append((k, v))
        return waits

    def _commit(self, tok, reads, writes):
        for b in writes:
            b.writer = tok
            b.readers = []
        for b in reads:
            if b not in writes:
                b.readers.append(tok)

    def op(self, eng, fn, reads=(), writes=()):
        waits = self._collect(eng, reads, writes)
        self.cnt[eng] += 1
        tok = (eng, self.cnt[eng])
        self.ops[eng].append((fn, waits, eng, 1))
        self._commit(tok, reads, writes)
        return tok

    def dma(self, q, out, in_, reads=(), writes=(), sembuf=None):
        sb = sembuf or (writes[0] if writes else reads[0])
        key = self._dsem(sb)
        waits = self._collect(q, reads, writes)
        sb.dcnt += 16
        self.dtot[key] += 16
        tok = (key, sb.dcnt)

        def fn(e, out=out, in_=in_):
            return e.dma_start(out=out, in_=in_)

        self.ops[q].append((fn, waits, key, 16))
        self._commit(tok, reads, writes)
        return tok

    def barrier(self):
        for e in ENGS:
            waits = []
            seen = self.seen[e]
            for k in ("pe", "act", "dve", "pool"):
                v = self.cnt[k]
                if k == e and e == "pe":
                    continue
                if v > 0 and seen.get(k, 0) < v:
                    seen[k] = v
                    waits.append((k, v))
            for k, v in self.dtot.items():
                if v > 0 and seen.get(k, 0) < v:
                    seen[k] = v
                    waits.append((k, v))
            if waits:
                self.ops[e].append((None, waits, None, 0))

    def emit(self):
        nc = self.nc
        self.barrier()
        sems = self.sems
        ops = self.ops

        def run(eng_name, e):
            for (fn, waits, key, amt) in ops[eng_name]:
                for (k, v) in waits:
                    e.wait_ge(sems[k], v)
                if fn is None:
                    continue
                ins = fn(e)
                ins.then_inc(sems[key], amt)

        with nc.Block() as block:
            @block.sync
            def _(e):
                run("sp", e)

            @block.scalar
            def _(e):
                run("act", e)

            @block.vector
            def _(e):
                run("dve", e)

            @block.gpsimd
            def _(e):
                run("pool", e)

            @block.tensor
            def _(e):
                run("pe", e)


class StopBuild(Exception):
    pass


class Region:
    def __init__(self, t, nwords):
        self.t = t
        self.n = nwords
        self.off = 0
        self.peak = 0

    def alloc(self, free_elems, dt=F32):
        esz = 4 if dt in (F32, I32) else 2
        words = (free_elems * esz + 3) // 4
        ap = self.t[:, self.off:self.off + words]
        self.off += words
        self.peak = max(self.peak, self.off)
        assert self.off <= self.n, f"region overflow {self.off} > {self.n}"
        return ap if dt == F32 else ap.bitcast(dt)


D = 1024
T_OWN = 2048
NT = 16
NPRE = 16
P = 128
OFF_K, OFF_V, OFF_LX, OFF_LG = 512, 640, 768, 1280
WIN = 1792
EPS = 1e-6
NEG = -1.0e9
NE = 32
TWO_PI = 6.283185307179586
GELU_C = 1.5957691216057308

DEBUG_STOP = None


def build(nc, dbg=None):
    dt = nc.dram_tensor
    x_d = dt("xin", [4096, D], F32, kind="ExternalInput").ap()
    pos_d = dt("pos", [P, 17], I32, kind="ExternalInput").ap()
    pp_d = dt("pple", [T_OWN, 256], F32, kind="ExternalInput").ap()
    win_d = dt("w_in", [D, WIN], F32, kind="ExternalInput").ap()
    wout_d = dt("w_out", [D, D], F32, kind="ExternalInput").ap()
    wrt_d = dt("w_rt", [D, 36], F32, kind="ExternalInput").ap()
    brt_d = dt("b_rt", [1, 36], F32, kind="ExternalInput").ap()
    ne_decl = NE if dbg in (None, "fewexp") else 1
    wg_d = dt("wg", [ne_decl, D, 512], F32, kind="ExternalInput").ap()
    wu_d = dt("wu", [ne_decl, D, 512], F32, kind="ExternalInput").ap()
    wd_d = dt("wd", [ne_decl, 512, D], F32, kind="ExternalInput").ap()
    wpg_d = dt("w_pg", [D, D], F32, kind="ExternalInput").ap()
    wpp_d = dt("w_pp", [256, D], F32, kind="ExternalInput").ap()
    gains_d = dt("gains", [4, D], F32, kind="ExternalInput").ap()
    colp_d = dt("colp", [P, 4, 10], F32, kind="ExternalInput").ap()
    wabd_d = dt("wa_bd", [P, 4, P], F32, kind="ExternalInput").ap()
    wxbd_d = dt("wx_bd", [P, 4, P], F32, kind="ExternalInput").ap()
    sinks_d = dt("sinks", [1, 8], F32, kind="ExternalInput").ap()
    mask0_d = dt("mask0", [P, 256], F32, kind="ExternalInput").ap()
    mask1_d = dt("mask1", [P, 256], F32, kind="ExternalInput").ap()
    hasp_d = dt("hasprev", [P, 1], F32, kind="ExternalInput").ap()
    ident_d = dt("ident", [P, P], F32, kind="ExternalInput").ap()
    invf_d = dt("invf", [P, 32], F32, kind="ExternalInput").ap()
    out_d = dt("out", [T_OWN, D], F32, kind="ExternalOutput").ap()

    with ExitStack() as st:
        S = Sched(nc, st)
        sb = lambda name, shape, dtp=F32: st.enter_context(nc.sbuf_tensor("sb_" + name, shape, dtp))
        B = S.buf

        X = sb("X", [P, NT, D])
        Xb = [B(f"X{i}") for i in range(NT)]
        gbc = sb("gbc", [P, D]); gbc_b = B("gbc")
        colp = sb("colp", [P, 4, 10]); colp_b = B("colp")
        der = sb("der", [P, 4, 8]); der_b = B("der")
        id16 = sb("id16", [P, P], BF16); id16_b = B("id16")
        id32 = sb("id32", [P, P]); id32_b = B("id32")
        ones16 = sb("ones16", [P, 2], BF16); ones_b = B("ones")
        sinks = sb("sinksb", [P, 8]); sinks_b = B("sinks")
        hasp = sb("hasp", [P, 1]); hasp_b = B("hasp")
        small = sb("small", [P, 64]); small_b = B("small")
        scr16 = sb("scr16", [P, D], BF16); scr_b = B("scr16")
        RW = 34900
        Rt = sb("R", [P, RW])
        R = Region(Rt, RW)

        ps = [st.enter_context(nc.psum_tensor(f"ps{i}", [P, 512], F32)) for i in range(8)]
        pb = [B(f"ps{i}") for i in range(8)]

        S.dma("sp", colp[:], colp_d, writes=[colp_b])
        S.dma("sp", id32[:], ident_d, writes=[id32_b])
        S.dma("pool", id16[:], ident_d, writes=[id16_b])
        S.dma("sp", sinks[:], sinks_d.partition_broadcast(P), writes=[sinks_b])
        S.dma("sp", hasp[:], hasp_d, writes=[hasp_b])
        S.dma("sp", gbc[:], gains_d[0:1, :].partition_broadcast(P), writes=[gbc_b])
        S.op("dve", lambda e: e.memset(ones16[:], 1.0), writes=[ones_b])

        S.op("act", lambda e: e.activation(der[:, :, 4], colp[:, :, 7], AF.Exp, scale=-1.0),
             reads=[colp_b], writes=[der_b])
        S.op("act", lambda e: e.activation(der[:, :, 5], der[:, :, 4], AF.Ln, bias=1.0),
             reads=[der_b], writes=[der_b])
        S.op("dve", lambda e: e.tensor_scalar(der[:, :, 0], der[:, :, 5], -8.0, None, ALU.mult),
             reads=[der_b], writes=[der_b])
        S.op("dve", lambda e: e.tensor_scalar(der[:, :, 1], der[:, :, 5], -16.0, None, ALU.mult),
             reads=[der_b], writes=[der_b])
        S.op("dve", lambda e: e.tensor_scalar(der[:, :, 2], colp[:, :, 5], -1.0, None, ALU.mult),
             reads=[colp_b, der_b], writes=[der_b])
        S.op("dve", lambda e: e.tensor_scalar(der[:, :, 3], colp[:, :, 6], -1.0, None, ALU.mult),
             reads=[colp_b, der_b], writes=[der_b])

        Win = R.alloc(8 * WIN, BF16).rearrange("p (c n) -> p c n", c=8); Win_b = B("Win")
        Wout = R.alloc(8 * D, BF16).rearrange("p (c n) -> p c n", c=8); Wout_b = B("Wout")
        wabd = R.alloc(4 * P, BF16).rearrange("p (c n) -> p c n", c=4)
        wxbd = R.alloc(4 * P, BF16).rearrange("p (c n) -> p c n", c=4)
        wbd_b = B("wbd")
        hT = R.alloc(8 * 512, BF16).rearrange("p (c t) -> p c t", c=8); hT_b = B("hT")
        hbf = R.alloc(D, BF16); hbf_b = B("hbf")
        hbf2 = R.alloc(D, BF16); hbf2_b = B("hbf2")
        smn0 = R.alloc(4); smn0_b = B("smn0"); smn1 = R.alloc(4); smn1_b = B("smn1")
        lxb = R.alloc(4 * 515).rearrange("p (c t) -> p c t", c=4); lxb_b = [B(f"lxb{c}") for c in range(4)]
        state = R.alloc(4); state_b = B("state")
        cvs = [R.alloc(512) for _ in range(3)]; cvs_b = [B("cv0"), B("cv1"), B("cv2")]
        cvbs = [R.alloc(512, BF16) for _ in range(3)]; cvbs_b = [B("cvb0"), B("cvb1"), B("cvb2")]
        t2x = R.alloc(512); t2x_b = B("t2x"); t3x = R.alloc(512); t3x_b = B("t3x")
        NTMP = 7
        off_alias = R.off
        ang = R.alloc(17 * 32); angi = R.alloc(17 * 32, I32); ang2 = R.alloc(17 * 32)
        ang_b = B("ang")
        R.off = off_alias
        tmp = [R.alloc(512) for _ in range(NTMP)]; tmp_b = [B(f"tmp{i}") for i in range(NTMP)]
        hl = R.alloc(512); hl_b = B("hl")
        hl2 = R.alloc(512); hl2_b = B("hl2")
        yT = R.alloc(4 * 512, BF16).rearrange("p (c t) -> p c t", c=4); yT_b = B("yT")
        sqT = R.alloc(4 * 512, BF16).rearrange("p (c t) -> p c t", c=4); sqT_b = B("sqT")
        sslru = R.alloc(4); sslru_b = B("sslru")
        cos_t = R.alloc(17 * 32).rearrange("p (t j) -> p t j", t=17)
        sin_t = R.alloc(17 * 32).rearrange("p (t j) -> p t j", t=17)
        nsin_t = R.alloc(17 * 32).rearrange("p (t j) -> p t j", t=17)
        rope_b = B("rope")
        off_pos = R.off
        posi = R.alloc(17, I32); posf = R.alloc(17); pos_b = B("pos")
        invf = R.alloc(32); invf_b = B("invf")
        off_mask = R.off
        mask0 = R.alloc(256); mask1 = R.alloc(256); mask_b = B("mask")
        ones128 = Rt[:, off_pos:off_pos + 64].bitcast(BF16); ones128_b = B("ones128")
        rbc = Rt[:, off_mask:off_mask + 512]; rbc_b = B("rbc")
        qk_sb = R.alloc(640); qk_b = B("qk_sb")
        rA = R.alloc(640); rB = R.alloc(640); rt_b = B("ropetmp")
        qkr = R.alloc(640, BF16); qkr_b = B("qkr")
        qT = R.alloc(8 * P, BF16).rearrange("p (c t) -> p c t", c=8); qT_b = B("qT")
        kT = [R.alloc(2 * P, BF16).rearrange("p (c t) -> p c t", c=2) for _ in range(3)]
        kT_b = [B("kT0"), B("kT1"), B("kT2")]
        vv = [R.alloc(P, BF16) for _ in range(3)]; vv_b = [B("v0"), B("v1"), B("v2")]
        p_sbs = [R.alloc(2 * 256, BF16).rearrange("p (h k) -> p h k", h=2) for _ in range(2)]; p_bs = [B("p_sb0"), B("p_sb1")]
        pTs = [R.alloc(4 * P, BF16).rearrange("p (h t) -> p h t", h=4) for _ in range(2)]; pT_bs = [B("pT0"), B("pT1")]
        maskb0 = R.alloc(256, BF16); maskb1 = R.alloc(256, BF16); maskb_b = B("maskb")
        sinks8 = R.alloc(8)
        sm2 = R.alloc(64); sm2_b = B("sm2")
        attn = R.alloc(512); attn_b = B("attn")
        attnb = R.alloc(512, BF16); attnb_b = B("attnb")
        attnT = R.alloc(4 * P, BF16).rearrange("p (c t) -> p c t", c=4); attnT_b = B("attnT")
        sm = R.alloc(64); sm_b = B("sm")
        rinv8t = R.alloc(8); rinv_b = B("rinv")
        print("phase1 region words", R.off, "of", RW)

        S.dma("pool", Win[:, :, OFF_LX:OFF_LG], win_d[:, OFF_LX:OFF_LG].rearrange("(c p) n -> p c n", p=P), writes=[Win_b])
        S.dma("pool", wabd[:], wabd_d, writes=[wbd_b])
        S.dma("pool", wxbd[:], wxbd_d, writes=[wbd_b])
        S.dma("pool", Win[:, :, 0:OFF_LX], win_d[:, 0:OFF_LX].rearrange("(c p) n -> p c n", p=P), writes=[Win_b])
        S.dma("pool", Win[:, :, OFF_LG:WIN], win_d[:, OFF_LG:WIN].rearrange("(c p) n -> p c n", p=P), writes=[Win_b])
        S.dma("sp", posi[:], pos_d, writes=[pos_b])
        S.dma("sp", invf[:], invf_d, writes=[invf_b])
        S.dma("sp", mask0[:], mask0_d, writes=[mask_b])
        S.dma("sp", mask1[:], mask1_d, writes=[mask_b])

        S.op("dve", lambda e: e.tensor_scalar(maskb0[:], mask0[:], 8.0, None, ALU.mult), reads=[mask_b], writes=[maskb_b])
        S.op("dve", lambda e: e.tensor_scalar(maskb1[:], mask1[:], 8.0, None, ALU.mult), reads=[mask_b], writes=[maskb_b])
        S.op("dve", lambda e: e.tensor_scalar(sinks8[:], sinks[:], 8.0, None, ALU.mult), reads=[sinks_b], writes=[sinks_b])
        S.op("dve", lambda e: e.tensor_copy(posf[:], posi[:]), reads=[pos_b], writes=[pos_b])
        ang3 = ang.rearrange("p (t j) -> p t j", t=17)
        for t in range(17):
            S.op("dve", lambda e, t=t: e.tensor_scalar(ang3[:, t, :], invf[:], posf[:, t:t + 1], None, ALU.mult),
                 reads=[pos_b, invf_b], writes=[ang_b])

        def sincos(dst, shift):
            S.op("dve", lambda e: e.tensor_scalar(ang2[:], ang[:], shift, 1.0 / TWO_PI, ALU.add, ALU.mult),
                 reads=[ang_b], writes=[rt_b])
            S.op("dve", lambda e: e.tensor_copy(angi[:], ang2[:]), reads=[rt_b], writes=[rt_b])
            S.op("dve", lambda e: e.tensor_copy(ang2[:], angi[:]), reads=[rt_b], writes=[rt_b])
            S.op("dve", lambda e: e.scalar_tensor_tensor(ang2[:], ang2[:], -TWO_PI, ang[:], ALU.mult, ALU.add),
                 reads=[rt_b, ang_b], writes=[rt_b])
            S.op("dve", lambda e: e.tensor_scalar(ang2[:], ang2[:], shift, 3.14159, ALU.add, ALU.min),
                 reads=[rt_b], writes=[rt_b])
            S.op("dve", lambda e: e.tensor_scalar(ang2[:], ang2[:], -3.14159, None, ALU.max),
                 reads=[rt_b], writes=[rt_b])
            S.op("act", lambda e: e.activation(dst.rearrange("p t j -> p (t j)"), ang2[:], AF.Sin),
                 reads=[rt_b], writes=[rope_b])

        sincos(sin_t, 0.0)
        sincos(cos_t, 1.5707963267948966)
        S.op("dve", lambda e: e.tensor_scalar(nsin_t[:, :, :], sin_t[:, :, :], -1.0, None, ALU.mult),
             reads=[rope_b], writes=[rope_b])
        S.barrier()
        S.op("dve", lambda e: e.memset(ones128[:, :], 1.0), writes=[ones128_b])


        def finish_dbg():
            for ot_ in range(NT):
                S.dma("sp", out_d[ot_ * P:(ot_ + 1) * P, :], X[:, ot_, :], reads=[Xb[ot_]])
            S.emit()

        if dbg == "c0":
            finish_dbg()
            return

        def rstd_from_ss(ss_ap, dst_ap, n, rb, wb):
            S.op("act", lambda e: e.activation(dst_ap, ss_ap, AF.Ln, scale=1.0 / n, bias=eps_ap),
                 reads=[rb, eps_b], writes=[wb])
            S.op("act", lambda e: e.activation(dst_ap, dst_ap, AF.Exp, scale=-0.5), reads=[wb], writes=[wb])

        epst = sb("epst", [P, 1]); eps_b = B("eps")
        eps_ap = epst[:, 0:1]
        S.op("dve", lambda e: e.memset(epst[:], EPS), writes=[eps_b])

        psT0 = ps[0][:, :].bitcast(BF16)
        psT1 = ps[1][:, :].bitcast(BF16)
        fm = [ps[2], ps[3], ps[6], ps[7]]
        fm_b = [pb[2], pb[3], pb[6], pb[7]]
        fmi = [0]

        def nextfm():
            i = fmi[0] % 4
            fmi[0] += 1
            return fm[i], fm_b[i]

        hbfs = [hbf, hbf2]; hbf_bs = [hbf_b, hbf2_b]
        scrs = [scr16, scr16]; scr_bs = [scr_b, scr_b]
        smn = [smn0, smn1]; smn_b = [smn0_b, smn1_b]

        def norm1(xap, xbuf, tcol):
            z = tcol % 2
            hb = hbfs[z]; hb_b = hbf_bs[z]; sc = scrs[z]; sc_b = scr_bs[z]; smz = smn[z]; smz_b = smn_b[z]
            S.op("act", lambda e: e.activation(sc[:], xap, AF.Square, accum_out=smz[:, 0:1]),
                 reads=[xbuf], writes=[sc_b, smz_b])
            rstd_from_ss(smz[:, 0:1], smz[:, 1:2], float(D), smz_b, smz_b)
            S.op("dve", lambda e: e.scalar_tensor_tensor(hb[:], xap, smz[:, 1:2], gbc[:], ALU.mult, ALU.mult),
                 reads=[xbuf, smz_b, gbc_b], writes=[hb_b])

        def norm2(tcol):
            z = tcol % 2
            hb = hbfs[z]; hb_b = hbf_bs[z]
            psTz = ps[z][:, :].bitcast(BF16)

            def tr(e):
                ins = None
                for c in range(8):
                    ins = e.transpose(psTz[:, c * P:(c + 1) * P], hb[:, c * P:(c + 1) * P], id16[:])
                return ins
            S.op("pe", tr, reads=[hb_b, id16_b], writes=[pb[z]])
            S.op("act", lambda e: e.copy(hT[:, :, tcol * P:(tcol + 1) * P],
                                         psTz.rearrange("p (c t) -> p c t", c=8)),
                 reads=[pb[z]], writes=[hT_b])

        def norm_transpose_tile(xap, xbuf, tcol):
            norm1(xap, xbuf, tcol)
            norm2(tcol)

        hls = [hl, hl2]; hl_bs = [hl_b, hl2_b]
        FMB = [3, 6, 7, 4, 5, 1]
        LB = [2, 0]
        lbk = [0]

        def nextL():
            i = LB[lbk[0] % 2]
            lbk[0] += 1
            return ps[i], pb[i]
        fmk = [0]

        def nextbank():
            i = FMB[fmk[0] % len(FMB)]
            fmk[0] += 1
            return ps[i], pb[i]

        def lru_block(blk, own):
            t0, t1, t2, t3, t4, t5, t6 = tmp
            b0, b1, b2, b3, b4, b5, b6 = tmp_b
            gates = {}

            def stageA(cc):
                cv = cvs[cc % 3]; cv_b = cvs_b[cc % 3]; cvb = cvbs[cc % 3]; cvb_b = cvbs_b[cc % 3]
                pl, plb = nextL()

                def mm(e, cc=cc, pl=pl):
                    ins = None
                    for c in range(8):
                        ins = e.matmul(pl[:, :], lhsT=Win[:, c, OFF_LX + cc * P:OFF_LX + (cc + 1) * P],
                                       rhs=hT[:, c, :], start=(c == 0), stop=(c == 7))
                    return ins
                S.op("pe", mm, reads=[Win_b, hT_b], writes=[plb])
                if blk == 0:
                    S.op("dve", lambda e, cc=cc: e.memset(lxb[:, cc, 0:3], 0.0), writes=[lxb_b[cc]])
                else:
                    S.op("dve", lambda e, cc=cc: e.tensor_copy(lxb[:, cc, 0:3], lxb[:, cc, 512:515]),
                         reads=[lxb_b[cc]], writes=[lxb_b[cc]])
                S.op("act", lambda e, cc=cc, pl=pl: e.copy(lxb[:, cc, 3:515], pl[:, :]),
                     reads=[plb], writes=[lxb_b[cc]])
                S.op("dve", lambda e, cc=cc, cv=cv: e.tensor_scalar(cv[:], lxb[:, cc, 0:512], colp[:, cc, 0:1],
                                                                     colp[:, cc, 4:5], ALU.mult, ALU.add),
                     reads=[lxb_b[cc], colp_b], writes=[cv_b])
                for j in range(1, 4):
                    S.op("dve", lambda e, cc=cc, j=j, cv=cv: e.scalar_tensor_tensor(
                        cv[:], lxb[:, cc, j:j + 512], colp[:, cc, j:j + 1], cv[:], ALU.mult, ALU.add),
                        reads=[lxb_b[cc], colp_b, cv_b], writes=[cv_b])
                S.op("act", lambda e, cv=cv, cvb=cvb: e.copy(cvb[:], cv[:]), reads=[cv_b], writes=[cvb_b])
                pga, pgab = nextbank()
                S.op("pe", lambda e, cc=cc, pga=pga, cvb=cvb: e.matmul(pga[:, :], lhsT=wabd[:, cc, :], rhs=cvb[:],
                                                                        start=True, stop=True),
                     reads=[wbd_b, cvb_b], writes=[pgab])
                pgx, pgxb = nextbank()
                S.op("pe", lambda e, cc=cc, pgx=pgx, cvb=cvb: e.matmul(pgx[:, :], lhsT=wxbd[:, cc, :], rhs=cvb[:],
                                                                        start=True, stop=True),
                     reads=[wbd_b, cvb_b], writes=[pgxb])
                gates[cc] = (pga, pgab, pgx, pgxb)

            def stageB(cc):
                cv = cvs[cc % 3]; cv_b = cvs_b[cc % 3]
                hlc = hls[cc % 2]; hlc_b = hl_bs[cc % 2]
                pga, pgab, pgx, pgxb = gates[cc][:4]
                t2 = [tmp[2], t2x][cc % 2]; b2 = [tmp_b[2], t2x_b][cc % 2]
                t3 = [tmp[3], t3x][cc % 2]; b3 = [tmp_b[3], t3x_b][cc % 2]
                S.op("act", lambda e, cc=cc, pga=pga: e.activation(t0[:], pga[:, :], AF.Exp, scale=-1.0,
                                                                    bias=der[:, cc, 2:3]),
                     reads=[pgab, der_b], writes=[b0])
                S.op("act", lambda e, cc=cc, pgx=pgx: e.activation(t4[:], pgx[:, :], AF.Exp, scale=-1.0,
                                                                    bias=der[:, cc, 3:4]),
                     reads=[pgxb, der_b], writes=[b4])
                S.op("act", lambda e: e.activation(t0[:], t0[:], AF.Ln, bias=1.0), reads=[b0], writes=[b0])
                S.op("act", lambda e: e.activation(t1[:], t0[:], AF.Exp, scale=-1.0), reads=[b0], writes=[b1])
                S.op("act", lambda e, cc=cc: e.activation(t2[:], t1[:], AF.Exp, scale=der[:, cc, 0:1]),
                     reads=[b1, der_b], writes=[b2])
                S.op("act", lambda e, cc=cc: e.activation(t3[:], t1[:], AF.Exp, scale=der[:, cc, 1:2]),
                     reads=[b1, der_b], writes=[b3])
                S.op("act", lambda e: e.activation(t3[:], t3[:], AF.Ln, scale=-1.0, bias=1.0),
                     reads=[b3], writes=[b3])
                S.op("act", lambda e: e.activation(t4[:], t4[:], AF.Ln, bias=1.0), reads=[b4], writes=[b4])
                S.op("dve", lambda e: e.scalar_tensor_tensor(t3[:], t3[:], 0.5, t4[:], ALU.mult, ALU.subtract),
                     reads=[b3, b4], writes=[b3])
                S.op("act", lambda e: e.activation(t3[:], t3[:], AF.Exp), reads=[b3], writes=[b3])
                S.op("dve", lambda e, cv=cv: e.tensor_tensor(t3[:], t3[:], cv[:], ALU.mult), reads=[b3, cv_b], writes=[b3])
                if blk == 0:
                    S.op("dve", lambda e, cc=cc: e.memset(state[:, cc:cc + 1], 0.0), writes=[state_b])
                if blk == 4:
                    S.op("dve", lambda e, cc=cc: e.tensor_tensor(state[:, cc:cc + 1], state[:, cc:cc + 1],
                                                                  hasp[:, 0:1], ALU.mult),
                         reads=[state_b, hasp_b], writes=[state_b])
                S.op("dve", lambda e, cc=cc, hlc=hlc: e.tensor_tensor_scan(hlc[:], t2[:], t3[:], state[:, cc:cc + 1],
                                                                            ALU.mult, ALU.add),
                     reads=[b2, b3, state_b], writes=[hlc_b])
                S.op("dve", lambda e, cc=cc, hlc=hlc: e.tensor_copy(state[:, cc:cc + 1], hlc[:, 511:512]),
                     reads=[hlc_b], writes=[state_b])

            def stageC(cc):
                hlc = hls[cc % 2]; hlc_b = hl_bs[cc % 2]
                plg, plgb = nextL()

                def mm2(e, cc=cc, plg=plg):
                    ins = None
                    for c in range(8):
                        ins = e.matmul(plg[:, :], lhsT=Win[:, c, OFF_LG + cc * P:OFF_LG + (cc + 1) * P],
                                       rhs=hT[:, c, :], start=(c == 0), stop=(c == 7))
                    return ins
                S.op("pe", mm2, reads=[Win_b, hT_b], writes=[plgb])
                S.op("act", lambda e, plg=plg: e.copy(t5[:], plg[:, :]), reads=[plgb], writes=[b5])
                S.op("dve", lambda e: e.tensor_tensor(t6[:], t5[:], t5[:], ALU.mult), reads=[b5], writes=[b6])
                S.op("dve", lambda e: e.tensor_scalar(t6[:], t6[:], 0.044715, 1.0, ALU.mult, ALU.add),
                     reads=[b6], writes=[b6])
                S.op("dve", lambda e: e.tensor_tensor(t6[:], t6[:], t5[:], ALU.mult), reads=[b6, b5], writes=[b6])
                S.op("act", lambda e: e.activation(t6[:], t6[:], AF.Exp, scale=-GELU_C), reads=[b6], writes=[b6])
                S.op("act", lambda e: e.activation(t6[:], t6[:], AF.Ln, bias=1.0), reads=[b6], writes=[b6])
                S.op("act", lambda e: e.activation(t6[:], t6[:], AF.Exp, scale=-1.0), reads=[b6], writes=[b6])
                S.op("dve", lambda e, hlc=hlc: e.tensor_tensor(t5[:], t5[:], hlc[:], ALU.mult), reads=[b5, hlc_b], writes=[b5])
                S.op("dve", lambda e: e.tensor_tensor(t5[:], t5[:], t6[:], ALU.mult), reads=[b5, b6], writes=[b5])
                S.op("act", lambda e, cc=cc: e.activation(sqT[:, cc, :], t5[:], AF.Square), reads=[b5], writes=[sqT_b])
                S.op("act", lambda e, cc=cc: e.copy(yT[:, cc, :], t5[:]), reads=[b5], writes=[yT_b])

            stageA(0)
            stageA(1)
            stageA(2)
            stageB(0)
            stageA(3)
            stageB(1)
            if own:
                stageC(0)
            stageB(2)
            if own:
                stageC(1)
            stageB(3)
            if own:
                stageC(2)
                stageC(3)
            if own:
                pss, pssb = nextL()

                def mmss(e, pss=pss):
                    ins = None
                    for cc in range(4):
                        ins = e.matmul(pss[:, :], lhsT=ones128[:, :], rhs=sqT[:, cc, :], start=(cc == 0), stop=(cc == 3))
                    return ins
                S.op("pe", mmss, reads=[sqT_b, ones128_b], writes=[pssb])
                S.op("act", lambda e, pss=pss: e.activation(rbc[:, :], pss[:, :], AF.Ln, scale=1.0 / 512.0, bias=eps_ap),
                     reads=[pssb, eps_b], writes=[rbc_b])
                S.op("act", lambda e: e.activation(rbc[:, :], rbc[:, :], AF.Exp, scale=-0.5), reads=[rbc_b], writes=[rbc_b])
                for cc in range(4):
                    S.op("dve", lambda e, cc=cc: e.tensor_tensor(yT[:, cc, :], yT[:, cc, :], rbc[:, :], ALU.mult),
                         reads=[yT_b, rbc_b], writes=[yT_b])

        def kv_tile1(gt, tcol, do_q):
            ti = gt - 15
            slot = gt % 3
            if do_q:
                def mmq(e):
                    ins = None
                    for c in range(8):
                        ins = e.matmul(ps[4][:, :], lhsT=hT[:, c, tcol * P:(tcol + 1) * P], rhs=Win[:, c, 0:512],
                                       start=(c == 0), stop=(c == 7))
                    return ins
                S.op("pe", mmq, reads=[hT_b, Win_b], writes=[pb[4]])
                S.op("act", lambda e: e.copy(qk_sb[:, 0:512], ps[4][:, :]), reads=[pb[4]], writes=[qk_b])

            def mmkv(e):
                ins = None
                for c in range(8):
                    ins = e.matmul(ps[5][:, 0:256], lhsT=hT[:, c, tcol * P:(tcol + 1) * P], rhs=Win[:, c, 512:768],
                                   start=(c == 0), stop=(c == 7))
                return ins
            S.op("pe", mmkv, reads=[hT_b, Win_b], writes=[pb[5]])
            S.op("act", lambda e: e.copy(qk_sb[:, 512:640], ps[5][:, 0:128]), reads=[pb[5]], writes=[qk_b])
            S.op("act", lambda e: e.copy(vv[slot][:], ps[5][:, 128:256]), reads=[pb[5]], writes=[vv_b[slot]])

            h0 = 0 if do_q else 8
            nh = 10 - h0
            v4 = lambda ap: ap.rearrange("p (h two j) -> p h two j", h=10, two=2)[:, h0:10, :, :]
            src4 = v4(qk_sb); A4 = v4(rA); B4 = v4(rB); dst4 = v4(qkr)
            cb = cos_t[:, ti, :].unsqueeze(1).unsqueeze(1).to_broadcast([P, nh, 2, 32])
            sb_ = sin_t[:, ti, :].unsqueeze(1).to_broadcast([P, nh, 32])
            nsb = nsin_t[:, ti, :].unsqueeze(1).to_broadcast([P, nh, 32])
            S.op("dve", lambda e: e.tensor_tensor(A4, src4, cb, ALU.mult), reads=[qk_b, rope_b], writes=[rt_b])
            S.op("dve", lambda e: e.tensor_tensor(B4[:, :, 0, :], src4[:, :, 1, :], nsb, ALU.mult),
                 reads=[qk_b, rope_b], writes=[rt_b])
            S.op("dve", lambda e: e.tensor_tensor(B4[:, :, 1, :], src4[:, :, 0, :], sb_, ALU.mult),
                 reads=[qk_b, rope_b], writes=[rt_b])
            S.op("dve", lambda e: e.tensor_tensor(dst4, A4, B4, ALU.add), reads=[rt_b], writes=[qkr_b])

        def kv_tile2(gt, tcol, do_q):
            slot = gt % 3

            def trk(e):
                ins = None
                for kvh in range(2):
                    ins = e.transpose(psT1[0:64, kvh * P:(kvh + 1) * P], qkr[:, (8 + kvh) * 64:(9 + kvh) * 64], id16[:])
                return ins
            S.op("pe", trk, reads=[qkr_b, id16_b], writes=[pb[1]])
            S.op("act", lambda e: e.copy(kT[slot][0:64, :, :], psT1[0:64, 0:2 * P].rearrange("p (c t) -> p c t", c=2)),
                 reads=[pb[1]], writes=[kT_b[slot]])
            if do_q:
                def trq(e):
                    ins = None
                    for h in range(8):
                        ins = e.transpose(psT0[0:64, h * P:(h + 1) * P], qkr[:, h * 64:(h + 1) * 64], id16[:])
                    return ins
                S.op("pe", trq, reads=[qkr_b, id16_b], writes=[pb[0]])
                S.op("act", lambda e: e.copy(qT[0:64, :, :], psT0[0:64, :].rearrange("p (c t) -> p c t", c=8)),
                     reads=[pb[0]], writes=[qT_b])

        def ck(name):
            if dbg == name:
                raise StopBuild()

        def attn_tile(gt, tcol):
            ot = gt - 16
            slot = gt % 3
            pslot = (gt - 1) % 3
            maskb = maskb0 if ot == 0 else maskb1
            def hp_ctx(hp):
                z = hp % 2
                return dict(kvh=hp // 2, z=z, p_sb=p_sbs[z], p_b=p_bs[z], pT=pTs[z], pT_b=pT_bs[z],
                            sbank=[6, 2][z], tbank=[1, 3][z], psS=ps[[6, 2][z]],
                            psTb=ps[[1, 3][z]][:, :].bitcast(BF16), smk=[sm, sm2][z], smk_b=[sm_b, sm2_b][z])

            def st_sc(hp):
                c = hp_ctx(hp)
                kvh, psS = c["kvh"], c["psS"]

                def mms(e):
                    ins = None
                    for hh in range(2):
                        h = hp * 2 + hh
                        ins = e.matmul(psS[:, hh * 256:(hh + 1) * 256], lhsT=id16[:, :], rhs=maskb[:, :],
                                       start=True, stop=False)
                        ins = e.matmul(psS[:, hh * 256:hh * 256 + 128], lhsT=qT[0:64, h, :],
                                       rhs=kT[pslot][0:64, kvh, :], start=False, stop=False)
                        ins = e.matmul(psS[:, hh * 256 + 128:hh * 256 + 256], lhsT=qT[0:64, h, :],
                                       rhs=kT[slot][0:64, kvh, :], start=False, stop=True)
                    return ins
                S.op("pe", mms, reads=[qT_b, kT_b[0], kT_b[1], kT_b[2], id16_b, maskb_b], writes=[pb[c["sbank"]]])

            def st_smx(hp):
                c = hp_ctx(hp)
                psS, smk, smk_b, p_sb, p_b, sbank = c["psS"], c["smk"], c["smk_b"], c["p_sb"], c["p_b"], c["sbank"]
                mx = smk[:, 8:10]; nb = smk[:, 10:12]; es = smk[:, 12:14]; rs = smk[:, 16:18]
                rinv = rinv8t[:, hp * 2:hp * 2 + 2]
                S.op("dve", lambda e: e.tensor_reduce(mx, psS[:, :].rearrange("p (h k) -> p h k", h=2), AX.X, ALU.max),
                     reads=[pb[sbank]], writes=[smk_b])
                S.op("dve", lambda e: e.tensor_tensor(mx, mx, sinks8[:, hp * 2:hp * 2 + 2], ALU.max),
                     reads=[smk_b, sinks_b], writes=[smk_b])
                S.op("dve", lambda e: e.tensor_scalar(nb, mx, -0.125, None, ALU.mult), reads=[smk_b], writes=[smk_b])
                for hh in range(2):
                    S.op("act", lambda e, hh=hh: e.activation(
                        p_sb[:, hh, :], psS[:, hh * 256:(hh + 1) * 256], AF.Exp, scale=0.125, bias=nb[:, hh:hh + 1],
                        accum_out=rs[:, hh:hh + 1]),
                        reads=[pb[sbank], smk_b], writes=[p_b, smk_b])

            def st_smb(hp):
                c = hp_ctx(hp)
                smk, smk_b = c["smk"], c["smk_b"]
                nb = smk[:, 10:12]; es = smk[:, 12:14]; rs = smk[:, 16:18]
                rinv = rinv8t[:, hp * 2:hp * 2 + 2]
                S.op("dve", lambda e: e.tensor_tensor(es, sinks[:, hp * 2:hp * 2 + 2], nb, ALU.add),
                     reads=[smk_b, sinks_b], writes=[smk_b])
                S.op("act", lambda e: e.activation(es, es, AF.Exp), reads=[smk_b], writes=[smk_b])
                S.op("dve", lambda e: e.tensor_tensor(rs, rs, es, ALU.add), reads=[smk_b], writes=[smk_b])
                S.op("dve", lambda e: e.reciprocal(rinv, rs), reads=[smk_b], writes=[rinv_b])

            def st_tp(hp):
                c = hp_ctx(hp)
                p_sb, p_b, pT, pT_b, psTb, tbank = c["p_sb"], c["p_b"], c["pT"], c["pT_b"], c["psTb"], c["tbank"]

                def trp(e):
                    ins = None
                    for hh in range(2):
                        for kb in range(2):
                            ins = e.transpose(psTb[:, (hh * 2 + kb) * P:(hh * 2 + kb + 1) * P],
                                              p_sb[:, hh, kb * P:(kb + 1) * P], id16[:])
                    return ins
                S.op("pe", trp, reads=[p_b, id16_b], writes=[pb[tbank]])
                S.op("act", lambda e: e.copy(pT[:, :, :], psTb[:, 0:4 * P].rearrange("p (h t) -> p h t", h=4)),
                     reads=[pb[tbank]], writes=[pT_b])

            def st_pv(hp):
                c = hp_ctx(hp)
                kvh, pT, pT_b = c["kvh"], c["pT"], c["pT_b"]

                def mmo(e):
                    ins = None
                    for hh in range(2):
                        h = hp * 2 + hh
                        ins = e.matmul(ps[7][:, h * 64:(h + 1) * 64], lhsT=pT[:, hh * 2, :],
                                       rhs=vv[pslot][:, kvh * 64:(kvh + 1) * 64], start=True, stop=False)
                        ins = e.matmul(ps[7][:, h * 64:(h + 1) * 64], lhsT=pT[:, hh * 2 + 1, :],
                                       rhs=vv[slot][:, kvh * 64:(kvh + 1) * 64], start=False, stop=True)
                    return ins
                S.op("pe", mmo, reads=[pT_b, vv_b[0], vv_b[1], vv_b[2]], writes=[pb[7]])

            st_sc(0); st_sc(1); st_smx(0); st_tp(0); st_sc(2); st_smx(1); st_smb(0); st_pv(0); st_tp(1)
            st_sc(3); st_smx(2); st_smb(1); st_pv(1); st_tp(2); st_smx(3); st_smb(2); st_pv(2); st_tp(3); st_smb(3); st_pv(3)
            ck("A7")
            rinv8 = rinv8t[:, 0:8]
            S.op("dve", lambda e: e.tensor_tensor(attn.rearrange("p (h d) -> p h d", h=8),
                                                  ps[7][:, :].rearrange("p (h d) -> p h d", h=8),
                                                  rinv8.unsqueeze(2).to_broadcast([P, 8, 64]), ALU.mult),
                 reads=[pb[7], rinv_b], writes=[attn_b])
            ck("A8")
            S.op("act", lambda e: e.activation(scr16[:, 0:512], attn[:], AF.Square, accum_out=sm[:, 2:3]),
                 reads=[attn_b], writes=[scr_b, sm_b])
            rstd_from_ss(sm[:, 2:3], sm[:, 3:4], 512.0, sm_b, sm_b)
            S.op("dve", lambda e: e.tensor_scalar(attnb[:], attn[:], sm[:, 3:4], None, ALU.mult),
                 reads=[attn_b, sm_b], writes=[attnb_b])

            def tra(e):
                ins = None
                for c in range(4):
                    ins = e.transpose(psT0[:, c * P:(c + 1) * P], attnb[:, c * P:(c + 1) * P], id16[:])
                return ins
            S.op("pe", tra, reads=[attnb_b, id16_b], writes=[pb[0]])
            S.op("act", lambda e: e.copy(attnT[:, :, :], psT0[:, 0:4 * P].rearrange("p (c t) -> p c t", c=4)),
                 reads=[pb[0]], writes=[attnT_b])
            ck("A9")
            def mmoa(e):
                ins = None
                for dh in range(2):
                    for c in range(8):
                        lhs = attnT[:, c, :] if c < 4 else yT[:, c - 4, tcol * P:(tcol + 1) * P]
                        ins = e.matmul(ps[4 + dh][:, :], lhsT=lhs, rhs=Wout[:, c, dh * 512:(dh + 1) * 512],
                                       start=(c == 0), stop=(c == 7))
                return ins
            S.op("pe", mmoa, reads=[attnT_b, yT_b, Wout_b], writes=[pb[4], pb[5]])
            for dh in range(2):
                S.op("dve", lambda e, dh=dh: e.tensor_tensor(X[:, ot, dh * 512:(dh + 1) * 512],
                                                             X[:, ot, dh * 512:(dh + 1) * 512], ps[4 + dh][:, :], ALU.add),
                     reads=[pb[4 + dh], Xb[ot]], writes=[Xb[ot]])
            S.op("act", lambda e: e.activation(scr16[:], X[:, ot, :], AF.Square, accum_out=small[:, 40 + ot:41 + ot]),
                 reads=[Xb[ot]], writes=[scr_b, small_b])

        for blk in range(8):
            own = blk >= 4
            if blk == 2:
                for c in range(8):
                    S.dma("pool", Wout[:, c, :], wout_d[c * P:(c + 1) * P, :], writes=[Wout_b])
            if blk == 3:
                for c in range(8):
                    gcol = colp[:, c, 9:10] if c < 4 else colp[:, c - 4, 8:9]
                    S.op("dve", lambda e, c=c, gcol=gcol: e.tensor_scalar(Wout[:, c, :], Wout[:, c, :], gcol, None, ALU.mult),
                         reads=[Wout_b, colp_b], writes=[Wout_b])
            if dbg and dbg[0] == "A" and blk == 4:
                try:
                    S.dma("sp", X[:, 0, :], x_d[16 * P:17 * P, :], writes=[Xb[0]])
                    norm_transpose_tile(X[:, 0, :], Xb[0], 0)
                    lru_block(4, True)
                    kv_tile1(16, 0, True)
                    kv_tile2(16, 0, True)
                    attn_tile(16, 0)
                except StopBuild:
                    pass
                finish_dbg()
                return
            xi = [(blk * 4 + t) - (16 if own else 0) for t in range(4)]
            for t in range(4):
                S.dma("sp", X[:, xi[t], :], x_d[(blk * 4 + t) * P:(blk * 4 + t + 1) * P, :], writes=[Xb[xi[t]]])
            norm1(X[:, xi[0], :], Xb[xi[0]], 0)
            norm1(X[:, xi[1], :], Xb[xi[1]], 1)
            norm2(0)
            norm1(X[:, xi[2], :], Xb[xi[2]], 2)
            norm2(1)
            norm1(X[:, xi[3], :], Xb[xi[3]], 3)
            norm2(2)
            norm2(3)
            if dbg == "n%d" % blk:
                finish_dbg()
                return
            lru_block(blk, own)
            if dbg == "b%d" % blk:
                finish_dbg()
                return
            if blk == 3:
                kv_tile1(15, 3, False)
                kv_tile2(15, 3, False)
            if dbg == "k%d" % blk:
                finish_dbg()
                return
            if own:
                g0_ = blk * 4
                kv_tile1(g0_, 0, True)
                kv_tile2(g0_, 0, True)
                for t in range(4):
                    gt = g0_ + t
                    if t < 3:
                        kv_tile1(gt + 1, t + 1, True)
                    attn_tile(gt, t)
                    if t < 3:
                        kv_tile2(gt + 1, t + 1, True)

        if dbg == "p1":
            for ot in range(NT):
                S.dma("sp", out_d[ot * P:(ot + 1) * P, :], X[:, ot, :], reads=[Xb[ot]])
            S.emit()
            return

        S.barrier()
        R.off = 0
        h2T = R.alloc(8 * T_OWN, BF16).rearrange("p (c t) -> p c t", c=8); h2T_b = B("h2T")
        cw = R.alloc(NT * 32).rearrange("p (t e) -> p t e", t=NT); cw_b = B("cw")
        wgs = [R.alloc(8 * 512, BF16).rearrange("p (c f) -> p c f", c=8) for _ in range(2)]
        wus = [R.alloc(8 * 512, BF16).rearrange("p (c f) -> p c f", c=8) for _ in range(2)]
        wds = [R.alloc(4 * D, BF16).rearrange("p (c d) -> p c d", c=4) for _ in range(2)]
        wsl_b = [B("wslot0"), B("wslot1")]
        sgs = [R.alloc(512) for _ in range(2)]; sg_b = [B("sg0"), B("sg1")]
        hid = [R.alloc(4 * 512, BF16).rearrange("p (c t) -> p c t", c=4) for _ in range(2)]
        hid_b = [B("hid0"), B("hid1")]
        mark = R.off
        nE = NE if dbg != "fewexp" else 2
        for E in range(min(2, nE)):
            S.dma("pool", wgs[E][:], wg_d[E].rearrange("(c p) f -> p c f", p=P), writes=[wsl_b[E]])
            S.dma("pool", wus[E][:], wu_d[E].rearrange("(c p) f -> p c f", p=P), writes=[wsl_b[E]])
            S.dma("pool", wds[E][:], wd_d[E].rearrange("(c p) d -> p c d", p=P), writes=[wsl_b[E]])
        wrt = R.alloc(8 * 36).rearrange("p (c n) -> p c n", c=8); wrt_b = B("wrt")
        brt = R.alloc(36); brt_b = B("brt")
        h2s = [R.alloc(D) for _ in range(2)]; h2_bs = [B("h2a"), B("h2b")]
        h2T32s = [R.alloc(8 * P).rearrange("p (c t) -> p c t", c=8) for _ in range(2)]; h2T32_bs = [B("h2Ta"), B("h2Tb")]
        small2 = R.alloc(16); small2_b = B("small2")
        sq2 = R.alloc(D, BF16); sq2_b = B("sq2")
        lg = R.alloc(NT * 36).rearrange("p (t n) -> p t n", t=NT); lg_b = B("lg")
        rt1 = R.alloc(NT * 32); rt2 = R.alloc(NT * 32); rt3 = R.alloc(NT * 32)
        rtb = B("rtmp")
        S.dma("sp", gbc[:], gains_d[1:2, :].partition_broadcast(P), writes=[gbc_b])
        S.dma("sp", wrt[:], wrt_d.rearrange("(c p) n -> p c n", p=P), writes=[wrt_b])
        S.dma("sp", brt[:], brt_d.partition_broadcast(P), writes=[brt_b])
        ssq = small[:, 40:56]; ssq_b = small_b
        rstd_from_ss(ssq, ssq, float(D), ssq_b, ssq_b)

        def q1(ot):
            z = ot % 2
            h2 = h2s[z]; h2_b = h2_bs[z]
            S.op("dve", lambda e: e.scalar_tensor_tensor(h2[:], X[:, ot, :], ssq[:, ot:ot + 1], gbc[:], ALU.mult, ALU.mult),
                 reads=[Xb[ot], ssq_b, gbc_b], writes=[h2_b])

        def q2(ot):
            z = ot % 2
            h2 = h2s[z]; h2_b = h2_bs[z]; h2T32 = h2T32s[z]; h2T32_b = h2T32_bs[z]
            for hf in range(2):
                bk = z * 2 + hf

                def trf(e, hf=hf, bk=bk):
                    ins = None
                    for c in range(4):
                        cg = hf * 4 + c
                        ins = e.transpose(ps[bk][:, c * P:(c + 1) * P], h2[:, cg * P:(cg + 1) * P], id32[:])
                    return ins
                S.op("pe", trf, reads=[h2_b, id32_b], writes=[pb[bk]])
                S.op("act", lambda e, hf=hf, bk=bk: e.copy(h2T32[:, hf * 4:hf * 4 + 4, :],
                                                           ps[bk][:, :].rearrange("p (c t) -> p c t", c=4)),
                     reads=[pb[bk]], writes=[h2T32_b])
            S.op("act", lambda e: e.copy(h2T[:, :, ot * P:(ot + 1) * P], h2T32[:, :, :]),
                 reads=[h2T32_b], writes=[h2T_b])

            def mmr(e):
                ins = None
                for c in range(8):
                    ins = e.matmul(ps[4 + z][:, 0:36], lhsT=h2T32[:, c, :], rhs=wrt[:, c, :], start=(c == 0), stop=(c == 7))
                return ins
            S.op("pe", mmr, reads=[h2T32_b, wrt_b], writes=[pb[4 + z]])
            S.op("dve", lambda e: e.tensor_tensor(lg[:, ot, :], ps[4 + z][:, 0:36], brt[:], ALU.add),
                 reads=[pb[4 + z], brt_b], writes=[lg_b])

        q1(0)
        for ot in range(NT):
            if ot + 1 < NT:
                q1(ot + 1)
            q2(ot)

        gl = lg[:, :, 0:4]
        el = lg[:, :, 4:36].rearrange("p t (g e) -> p t g e", g=4)
        gmax = small[:, 8:24]
        gm = rt1[:, 0:64].rearrange("p (t g) -> p t g", t=NT)
        gex = rt1[:, 64:128].rearrange("p (t g) -> p t g", t=NT)
        gtp = small[:, 24:40]
        esel = rt2[:, 0:128].rearrange("p (t e) -> p t e", t=NT)
        etmp = rt2[:, 128:256].rearrange("p (t e) -> p t e", t=NT)
        m1 = rt1[:, 128:144]; m2 = rt1[:, 144:160]; p1 = rt1[:, 160:176]; w1 = rt1[:, 176:192]; w2 = rt1[:, 192:208]
        mk1 = rt3[:, 0:128].rearrange("p (t e) -> p t e", t=NT)
        mk2 = rt3[:, 128:256].rearrange("p (t e) -> p t e", t=NT)
        e2 = rt3[:, 256:384].rearrange("p (t e) -> p t e", t=NT)
        ew = rt2[:, 256:384].rearrange("p (t e) -> p t e", t=NT)
        RB = [lg_b, rtb, small_b]

        def dv(fn):
            S.op("dve", fn, reads=RB, writes=[rtb, small_b])

        bc3 = lambda ap2, n: ap2.unsqueeze(2).to_broadcast([P, NT, n])
        dv(lambda e: e.tensor_reduce(gmax, gl, AX.X, ALU.max))
        dv(lambda e: e.tensor_tensor(gm, gl, bc3(gmax, 4), ALU.is_equal))
        dv(lambda e: e.tensor_tensor(gex, gl, bc3(gmax, 4), ALU.subtract))
        S.op("act", lambda e: e.activation(gex, gex, AF.Exp), reads=[rtb], writes=[rtb])
        dv(lambda e: e.tensor_reduce(gtp, gex, AX.X, ALU.add))
        dv(lambda e: e.reciprocal(gtp, gtp))
        for g in range(4):
            if g == 0:
                dv(lambda e: e.tensor_tensor(esel, el[:, :, 0, :], bc3(gm[:, :, 0], 8), ALU.mult))
            else:
                dv(lambda e, g=g: e.tensor_tensor(etmp, el[:, :, g, :], bc3(gm[:, :, g], 8), ALU.mult))
                dv(lambda e: e.tensor_tensor(esel, esel, etmp, ALU.add))
        dv(lambda e: e.tensor_reduce(m1, esel, AX.X, ALU.max))
        dv(lambda e: e.tensor_tensor(mk1, esel, bc3(m1, 8), ALU.is_equal))
        dv(lambda e: e.scalar_tensor_tensor(e2, mk1, -1.0e30, esel, ALU.mult, ALU.add))
        dv(lambda e: e.tensor_reduce(m2, e2, AX.X, ALU.max))
        dv(lambda e: e.tensor_tensor(mk2, e2, bc3(m2, 8), ALU.is_equal))
        dv(lambda e: e.tensor_tensor(p1, m2, m1, ALU.subtract))
        S.op("act", lambda e: e.activation(p1, p1, AF.Exp), reads=[rtb], writes=[rtb])
        dv(lambda e: e.tensor_scalar(p1, p1, 1.0, None, ALU.add))
        dv(lambda e: e.reciprocal(p1, p1))
        dv(lambda e: e.tensor_tensor(w1, p1, gtp, ALU.mult))
        dv(lambda e: e.tensor_tensor(w2, gtp, w1, ALU.subtract))
        dv(lambda e: e.tensor_tensor(ew, mk1, bc3(w1, 8), ALU.mult))
        dv(lambda e: e.tensor_tensor(etmp, mk2, bc3(w2, 8), ALU.mult))
        dv(lambda e: e.tensor_tensor(ew, ew, etmp, ALU.add))
        cw4 = cw.rearrange("p t (g e) -> p t g e", g=4)
        for g in range(4):
            S.op("dve", lambda e, g=g: e.tensor_tensor(cw4[:, :, g, :], ew, bc3(gm[:, :, g], 8), ALU.mult),
                 reads=[rtb], writes=[cw_b])

        if dbg == "p2":
            S.op("dve", lambda e: e.tensor_copy(X[:, 0, 0:512], cw.rearrange("p t e -> p (t e)")),
                 reads=[cw_b, Xb[0]], writes=[Xb[0]])
            for ot in range(NT):
                S.dma("sp", out_d[ot * P:(ot + 1) * P, :], X[:, ot, :], reads=[Xb[ot]])
            S.emit()
            return

        save_off = R.off
        R.off = mark
        Wpg = R.alloc(8 * D, BF16).rearrange("p (c n) -> p c n", c=8); Wpg_b = B("Wpg")
        Wpp = R.alloc(2 * D, BF16).rearrange("p (c n) -> p c n", c=2); Wpp_b = B("Wpp")
        assert R.off <= save_off
        R.off = save_off
        p2_done = [cw_b, h2T_b, lg_b, rtb, small_b, small2_b, ssq_b, sq2_b, scr_b]
        print("phase3 region words", R.off, "of", RW)
        step = 0
        pending = [None]

        def emit_down(s_, hs, tb, E):
            for t in range(4):
                ot = tb * 4 + t
                for dh in range(2):
                    bank = 4 + (t % 2) * 2 + dh

                    def mmd(e, t=t, dh=dh, bank=bank):
                        ins = None
                        for fc in range(4):
                            ins = e.matmul(ps[bank][:, :], lhsT=hid[hs][:, fc, t * P:(t + 1) * P],
                                           rhs=wds[s_][:, fc, dh * 512:(dh + 1) * 512], start=(fc == 0), stop=(fc == 3))
                        return ins
                    S.op("pe", mmd, reads=[hid_b[hs], wsl_b[s_]], writes=[pb[bank]])
                    S.op("dve", lambda e, ot=ot, dh=dh, bank=bank: e.scalar_tensor_tensor(
                        X[:, ot, dh * 512:(dh + 1) * 512], ps[bank][:, :], cw[:, ot, E:E + 1],
                        X[:, ot, dh * 512:(dh + 1) * 512], ALU.mult, ALU.add),
                        reads=[pb[bank], cw_b, Xb[ot]], writes=[Xb[ot]])

        for E in range(nE):
            s_ = E % 2
            if E == nE - 3 or (nE < 4 and E == 0):
                for c in range(8):
                    S.dma("pool", Wpg[:, c, :], wpg_d[c * P:(c + 1) * P, :], reads=p2_done, writes=[Wpg_b], sembuf=Wpg_b)
                S.dma("pool", Wpp[:], wpp_d.rearrange("(c p) n -> p c n", p=P), reads=p2_done, writes=[Wpp_b], sembuf=Wpp_b)
                S.dma("sp", gbc[:], gains_d[2:3, :].partition_broadcast(P), writes=[gbc_b])
            if E >= 2:
                S.dma("pool", wgs[s_][:], wg_d[E].rearrange("(c p) f -> p c f", p=P), writes=[wsl_b[s_]])
                S.dma("pool", wus[s_][:], wu_d[E].rearrange("(c p) f -> p c f", p=P), writes=[wsl_b[s_]])
                S.dma("pool", wds[s_][:], wd_d[E].rearrange("(c p) d -> p c d", p=P), writes=[wsl_b[s_]])
            for tb in range(4):
                hs = step % 2
                step += 1
                for fc in range(4):
                    gi = fc % 2

                    def mmg(e, s_=s_, fc=fc, tb=tb, gi=gi):
                        ins = None
                        for c in range(8):
                            ins = e.matmul(ps[gi][:, :], lhsT=wgs[s_][:, c, fc * P:(fc + 1) * P],
                                           rhs=h2T[:, c, tb * 512:(tb + 1) * 512], start=(c == 0), stop=(c == 7))
                        return ins
                    S.op("pe", mmg, reads=[wsl_b[s_], h2T_b], writes=[pb[gi]])

                    def mmu(e, s_=s_, fc=fc, tb=tb, gi=gi):
                        ins = None
                        for c in range(8):
                            ins = e.matmul(ps[2 + gi][:, :], lhsT=wus[s_][:, c, fc * P:(fc + 1) * P],
                                           rhs=h2T[:, c, tb * 512:(tb + 1) * 512], start=(c == 0), stop=(c == 7))
                        return ins
                    S.op("pe", mmu, reads=[wsl_b[s_], h2T_b], writes=[pb[2 + gi]])
                    S.op("act", lambda e, gi=gi: e.activation(sgs[gi][:], ps[gi][:, :], AF.Silu),
                         reads=[pb[gi]], writes=[sg_b[gi]])
                    S.op("dve", lambda e, gi=gi, hs=hs, fc=fc: e.tensor_tensor(hid[hs][:, fc, :], sgs[gi][:],
                                                                                  ps[2 + gi][:, :], ALU.mult),
                         reads=[sg_b[gi], pb[2 + gi]], writes=[hid_b[hs]])
                    if fc == 1 and pending[0] is not None:
                        emit_down(*pending[0])
                        pending[0] = None
                pending[0] = (s_, hs, tb, E)
        if pending[0] is not None:
            emit_down(*pending[0])
            pending[0] = None
        ss1 = small[:, 40:56]; ss1_b = small_b
        for ot in range(NT):
            S.op("act", lambda e, ot=ot: e.activation(scr16[:], X[:, ot, :], AF.Square, accum_out=ss1[:, ot:ot + 1]),
                 reads=[Xb[ot]], writes=[scr_b, ss1_b])

        S.barrier()
        R.off = 0
        gfin = R.alloc(D); gfin_b = B("gfin")
        h3a = [R.alloc(D, BF16) for _ in range(2)]; h3a_b = [B("h3a0"), B("h3a1")]
        h3T_all = R.alloc(NT * 8 * P, BF16).rearrange("p (t c k) -> p t c k", t=NT, c=8)
        h3T_ab = [B(f"h3T{t}") for t in range(NT)]
        pt32 = [R.alloc(256) for _ in range(2)]; pt32_b = [B("pt0"), B("pt1")]
        pt16s = [R.alloc(256, BF16) for _ in range(2)]; pt16_bs = [B("pt16a"), B("pt16b")]
        ppTs = [R.alloc(2 * P, BF16).rearrange("p (c t) -> p c t", c=2) for _ in range(2)]; ppT_bs = [B("ppTa"), B("ppTb")]
        sgates = [R.alloc(D) for _ in range(2)]; sgate_bs = [B("sga"), B("sgb")]
        sq4 = [R.alloc(D, BF16) for _ in range(2)]; sq4_b = [B("sq4a"), B("sq4b")]
        ob = [R.alloc(D) for _ in range(2)]; ob_b = [B("ob0"), B("ob1")]
        ss2 = R.alloc(16); ss2_b = B("ss2")
        assert R.off <= mark, (R.off, mark)
        S.dma("sp", gfin[:], gains_d[3:4, :].partition_broadcast(P), writes=[gfin_b])
        rstd_from_ss(ss1, ss1, float(D), ss1_b, ss1_b)

        def n4(ot):
            z = ot % 2
            S.op("dve", lambda e: e.scalar_tensor_tensor(h3a[z][:], X[:, ot, :], ss1[:, ot:ot + 1], gbc[:], ALU.mult, ALU.mult),
                 reads=[Xb[ot], ss1_b, gbc_b], writes=[h3a_b[z]])

        def t4(ot):
            z = ot % 2
            psTz = ps[z][:, :].bitcast(BF16)

            def tr3(e):
                ins = None
                for cc in range(8):
                    ins = e.transpose(psTz[:, cc * P:(cc + 1) * P], h3a[z][:, cc * P:(cc + 1) * P], id16[:])
                return ins
            S.op("pe", tr3, reads=[h3a_b[z], id16_b], writes=[pb[z]])
            S.op("act", lambda e: e.copy(h3T_all[:, ot, :, :], psTz.rearrange("p (c t) -> p c t", c=8)),
                 reads=[pb[z]], writes=[h3T_ab[ot]])

        n4(0)
        for ot in range(NT):
            if ot + 1 < NT:
                n4(ot + 1)
            t4(ot)

        psTp = ps[0][:, :].bitcast(BF16)

        def pA(ot):
            sl = ot % 2
            S.dma("sp", pt32[sl][:], pp_d[ot * P:(ot + 1) * P, :], writes=[pt32_b[sl]])
            S.op("act", lambda e: e.copy(pt16s[sl][:], pt32[sl][:]), reads=[pt32_b[sl]], writes=[pt16_bs[sl]])

            def trp2(e):
                ins = None
                for cc in range(2):
                    ins = e.transpose(psTp[:, cc * P:(cc + 1) * P], pt16s[sl][:, cc * P:(cc + 1) * P], id16[:])
                return ins
            S.op("pe", trp2, reads=[pt16_bs[sl], id16_b], writes=[pb[0]])
            S.op("act", lambda e: e.copy(ppTs[sl][:, :, :], psTp[:, 0:2 * P].rearrange("p (c t) -> p c t", c=2)),
                 reads=[pb[0]], writes=[ppT_bs[sl]])

        def pG(ot):
            sl = ot % 2
            for dh in range(2):
                gbk = 2 + sl * 2 + dh

                def mmpg(e, dh=dh, gbk=gbk):
                    ins = None
                    for cc in range(8):
                        ins = e.matmul(ps[gbk][:, :], lhsT=h3T_all[:, ot, cc, :], rhs=Wpg[:, cc, dh * 512:(dh + 1) * 512],
                                       start=(cc == 0), stop=(cc == 7))
                    return ins
                S.op("pe", mmpg, reads=[h3T_ab[ot], Wpg_b], writes=[pb[gbk]])

        def pP(ot):
            sl = ot % 2
            for dh in range(2):
                def mmpp(e, dh=dh):
                    ins = None
                    for cc in range(2):
                        ins = e.matmul(ps[6 + dh][:, :], lhsT=ppTs[sl][:, cc, :], rhs=Wpp[:, cc, dh * 512:(dh + 1) * 512],
                                       start=(cc == 0), stop=(cc == 1))
                    return ins
                S.op("pe", mmpp, reads=[ppT_bs[sl], Wpp_b], writes=[pb[6 + dh]])

        def pU(ot):
            sl = ot % 2
            sgate = sgates[sl]; sgate_b = sgate_bs[sl]
            for dh in range(2):
                gbk = 2 + sl * 2 + dh
                sgd = sgate[:, dh * 512:(dh + 1) * 512]
                S.op("act", lambda e, gbk=gbk, sgd=sgd: e.activation(sgd, ps[gbk][:, :], AF.Sigmoid),
                     reads=[pb[gbk]], writes=[sgate_b])
                S.op("dve", lambda e, dh=dh, sgd=sgd: e.tensor_tensor(sgd, sgd, ps[6 + dh][:, :], ALU.mult),
                     reads=[sgate_b, pb[6 + dh]], writes=[sgate_b])
            S.op("dve", lambda e: e.tensor_tensor(X[:, ot, :], X[:, ot, :], sgate[:], ALU.add),
                 reads=[sgate_b, Xb[ot]], writes=[Xb[ot]])
            S.op("act", lambda e: e.activation(sq4[sl][:], X[:, ot, :], AF.Square, accum_out=ss2[:, ot:ot + 1]),
                 reads=[Xb[ot]], writes=[sq4_b[sl], ss2_b])

        pA(0)
        pG(0)
        for ot in range(NT):
            if ot + 1 < NT:
                pA(ot + 1)
            pP(ot)
            if ot + 1 < NT:
                pG(ot + 1)
            pU(ot)
        rstd_from_ss(ss2[:], ss2[:], float(D), ss2_b, ss2_b)
        for ot in range(NT):
            sl = ot % 2
            S.op("dve", lambda e, ot=ot: e.scalar_tensor_tensor(X[:, ot, :], X[:, ot, :], ss2[:, ot:ot + 1], gfin[:],
                                                                ALU.mult, ALU.mult),
                 reads=[Xb[ot], ss2_b, gfin_b], writes=[Xb[ot]])
            S.dma("sp", out_d[ot * P:(ot + 1) * P, :], X[:, ot, :], reads=[Xb[ot]], sembuf=ob_b[ot % 2])
        S.emit()


def make_in_maps(inp, dbg=None):
    f = lambda a: np.ascontiguousarray(np.asarray(a), dtype=np.float32)
    x = f(inp["x"]); p = f(inp["p"])[0]
    positions = np.asarray(inp["positions"]).astype(np.int32)
    w_in = f(inp["w_in"])[0]; w_out = f(inp["w_out"])[0]
    w_rt = np.ascontiguousarray(np.concatenate([f(inp["w_router_group"])[0], f(inp["w_router_expert"])[0]], axis=1))
    b_rt = np.ascontiguousarray(np.concatenate([f(inp["b_router_group"])[0], f(inp["b_router_expert"])[0]], axis=0)[None, :])
    wg = np.ascontiguousarray(f(inp["w_expert_gate"])[0].reshape(NE, D, 512))
    wu = np.ascontiguousarray(f(inp["w_expert_up"])[0].reshape(NE, D, 512))
    wd = np.ascontiguousarray(f(inp["w_expert_down"])[0].reshape(NE, 512, D))
    w_pg = f(inp["w_ple_gate"])[0]; w_pp = f(inp["w_ple_proj"])[0]
    gains = np.ascontiguousarray(np.stack([f(inp["g_mix"])[0], f(inp["g_ffn"])[0], f(inp["g_ple"])[0], f(inp["g_final"])], axis=0))
    cols = [f(inp["conv_w"])[0][j] for j in range(4)] + [f(inp["conv_b"])[0], f(inp["lru_ba"])[0], f(inp["lru_bx"])[0],
                                                          f(inp["lru_lambda"])[0], f(inp["g_lru_out"])[0], f(inp["g_attn_out"])[0]]
    colp = np.ascontiguousarray(np.stack([c.reshape(4, P).T for c in cols], axis=2))
    wa = f(inp["lru_wa"])[0]; wx = f(inp["lru_wx"])[0]
    wa_bd = np.zeros((P, 4, P), np.float32); wx_bd = np.zeros((P, 4, P), np.float32)
    for cc in range(4):
        for hh in range(2):
            wa_bd[hh * 64:(hh + 1) * 64, cc, hh * 64:(hh + 1) * 64] = wa[2 * cc + hh]
            wx_bd[hh * 64:(hh + 1) * 64, cc, hh * 64:(hh + 1) * 64] = wx[2 * cc + hh]
    sinks = f(inp["sinks"])
    qi = np.arange(P)[:, None]; kj = np.arange(P)[None, :]
    maskc = np.where(kj <= qi, 0.0, NEG).astype(np.float32)
    maskp = np.where(kj > qi, 0.0, NEG).astype(np.float32)
    mask1 = np.ascontiguousarray(np.concatenate([maskp, maskc], axis=1))
    mask0_first = np.ascontiguousarray(np.concatenate([np.full((P, P), NEG, np.float32), maskc], axis=1))
    ident = np.eye(P, dtype=np.float32)
    invf = np.ascontiguousarray(np.broadcast_to(
        (10000.0 ** (-np.arange(0, 64, 2, dtype=np.float32) / 64.0)).astype(np.float32)[None, :], (P, 32)))
    if dbg not in (None, "fewexp"):
        wg, wu, wd = wg[:1], wu[:1], wd[:1]
    shared = dict(w_in=w_in, w_out=w_out, w_rt=w_rt, b_rt=b_rt, wg=wg, wu=wu, wd=wd, w_pg=w_pg, w_pp=w_pp,
                  gains=gains, colp=colp, wa_bd=wa_bd, wx_bd=wx_bd, sinks=sinks, mask1=mask1, ident=ident, invf=invf)
    maps = []
    for core in range(8):
        b, h = core // 2, core % 2
        xin = np.zeros((4096, D), np.float32)
        xin[2048:] = x[b, h * 2048:(h + 1) * 2048]
        pos17 = np.zeros((17, P), np.int32)
        pos17[1:] = positions[b, h * 2048:(h + 1) * 2048].reshape(16, P)
        if h == 1:
            xin[:2048] = x[b, 0:2048]
            pos17[0] = positions[b, 2048 - P:2048]
        m = dict(shared)
        m["xin"] = xin
        m["pos"] = np.ascontiguousarray(pos17.T)
        m["pple"] = np.ascontiguousarray(p[b, h * 2048:(h + 1) * 2048])
        m["mask0"] = mask1 if h == 1 else mask0_first
        m["hasprev"] = np.full((P, 1), float(h), np.float32)
        maps.append(m)
    return maps


def kernel(**inputs):
    nc = bass.Bass("TRN2", target_bir_lowering=False)
    build(nc, DEBUG_STOP)
    maps = make_in_maps(inputs)
    res = run_bass_kernel_spmd(nc, maps, core_ids=list(range(8)))
    out = np.zeros((4, 4096, D), np.float32)
    for core in range(8):
        b, h = core // 2, core % 2
        out[b, h * 2048:(h + 1) * 2048] = res.results[core]["out"]
    return out
```
